# Optimizing a Trainium2 kernel written in Bass

```python
import jax, jax.numpy as jnp
from jax import lax
import numpy as np

D_MODEL = 1024
BATCH = 8
SEQ = 2048
DEPTH = 1

CHUNK = 64
EPS = 1e-6
REC_WIDTH = D_MODEL // 2
REC_HEADS = 8
REC_HEAD_DIM = REC_WIDTH // REC_HEADS
CONV_WIDTH = 4
LRU_C = 8.0
ATT_HEAD_DIM = 64
ATT_HEADS = (D_MODEL // 2) // ATT_HEAD_DIM
ATT_WIDTH = ATT_HEADS * ATT_HEAD_DIM
LEFT_CHUNKS = 8
BAND = (LEFT_CHUNKS + 1) * CHUNK
MAX_REL = 128
MIX_WIDTH = REC_WIDTH + ATT_WIDTH
IN_WIDTH = 2 * REC_WIDTH + 3 * ATT_WIDTH
N_GROUPS = 4
EXPERTS_PER_GROUP = 4
N_EXPERTS = N_GROUPS * EXPERTS_PER_GROUP
TOP_K = 2
D_FF_EXPERT = D_MODEL // 2
DISPATCH_BLOCK = 128

kernel_name = 'hybrid_rglru_chunkattn_hiermoe'


def rms_norm(x, g):
    x32 = x.astype(jnp.float32)
    y = x32 * lax.rsqrt(jnp.mean(x32 * x32, axis=-1, keepdims=True) + EPS)
    return (y * g.astype(jnp.float32)).astype(x.dtype)


def rg_lru_group(u, gate, conv_w, conv_b, w_a, b_a, w_x, b_x, lam):
    B, S, _ = u.shape
    up = jnp.pad(u, ((0, 0), (CONV_WIDTH - 1, 0), (0, 0)))
    uc = conv_b
    for j in range(CONV_WIDTH):
        uc = uc + up[:, j:j + S] * conv_w[j]
    ub = uc.reshape(B, S, REC_HEADS, REC_HEAD_DIM)
    r = jax.nn.sigmoid(jnp.einsum('bshi,hij->bshj', ub, w_a) + b_a).reshape(B, S, REC_WIDTH)
    i = jax.nn.sigmoid(jnp.einsum('bshi,hij->bshj', ub, w_x) + b_x).reshape(B, S, REC_WIDTH)
    log_a = -LRU_C * r.astype(jnp.float32) * jax.nn.softplus(-lam.astype(jnp.float32))
    a = jnp.exp(log_a)
    bx = jnp.sqrt(-jnp.expm1(2.0 * log_a)) * (i * uc).astype(jnp.float32)

    def combine(left, right):
        a1, b1 = left
        a2, b2 = right
        return a1 * a2, a2 * b1 + b2

    _, h = lax.associative_scan(combine, (a, bx), axis=1)
    return h.astype(u.dtype) * jax.nn.gelu(gate)


def chunk_attention_group(q, k, v, rel_bias):
    B, S, _ = q.shape
    NC = S // CHUNK
    qc = q.reshape(B, NC, CHUNK, ATT_HEADS, ATT_HEAD_DIM) * (ATT_HEAD_DIM ** -0.5)
    pad = LEFT_CHUNKS * CHUNK

    def band(t):
        tc = jnp.pad(t.reshape(B, S, ATT_HEADS, ATT_HEAD_DIM), ((0, 0), (pad, 0), (0, 0), (0, 0)))
        tc = tc.reshape(B, NC + LEFT_CHUNKS, CHUNK, ATT_HEADS, ATT_HEAD_DIM)
        return jnp.concatenate([tc[:, j:j + NC] for j in range(LEFT_CHUNKS + 1)], axis=2)

    kb = band(k)
    vb = band(v)
    q_off = jnp.arange(CHUNK)
    k_off = jnp.arange(BAND)
    rel = jnp.clip(pad + q_off[:, None] - k_off[None, :], -MAX_REL, MAX_REL) + MAX_REL
    bias = rel_bias.astype(jnp.float32)[:, rel]
    key_pos = (jnp.arange(NC)[:, None] - LEFT_CHUNKS) * CHUNK + k_off[None, :]
    mask = jnp.where(key_pos >= 0, 0.0, -1e30).astype(jnp.float32)
    s = jnp.einsum('bcqhd,bckhd->bchqk', qc, kb, preferred_element_type=jnp.float32)
    s = s + bias[None, None] + mask[None, :, None, None, :]
    p = jax.nn.softmax(s, axis=-1).astype(v.dtype)
    o = jnp.einsum('bchqk,bckhd->bcqhd', p, vb)
    return o.reshape(B, S, ATT_WIDTH)


def hierarchical_moe(xn, w_group, b_group, w_router, b_router, w_e_gate, w_e_up, w_e_down):
    B, S, D = xn.shape
    T = B * S
    xt = xn.reshape(T, D)
    x32 = xt.astype(jnp.float32)
    group_logits = x32 @ w_group.astype(jnp.float32) + b_group.astype(jnp.float32)
    group_probs = jax.nn.softmax(group_logits, axis=-1)
    g = jnp.argmax(group_logits, axis=-1)
    p_g = jnp.take_along_axis(group_probs, g[:, None], axis=1)
    exp_logits = (x32 @ w_router.astype(jnp.float32) + b_router.astype(jnp.float32))
    exp_logits = exp_logits.reshape(T, N_GROUPS, EXPERTS_PER_GROUP)
    sel = jnp.take_along_axis(exp_logits, g[:, None, None], axis=1)[:, 0]
    top_v, top_i = lax.top_k(sel, TOP_K)
    weights = p_g * jax.nn.softmax(top_v, axis=-1)
    expert_id = g[:, None] * EXPERTS_PER_GROUP + top_i

    TK = T * TOP_K
    flat_e = expert_id.reshape(TK)
    flat_tok = jnp.repeat(jnp.arange(T), TOP_K)
    flat_w = weights.reshape(TK)
    order = jnp.argsort(flat_e)
    e_sorted = flat_e[order]
    tok_sorted = flat_tok[order]
    w_sorted = flat_w[order]
    counts = jnp.bincount(flat_e, length=N_EXPERTS)
    padded = (counts + DISPATCH_BLOCK - 1) // DISPATCH_BLOCK * DISPATCH_BLOCK
    start = jnp.cumsum(counts) - counts
    pend = jnp.cumsum(padded)
    pstart = pend - padded
    dest = pstart[e_sorted] + (jnp.arange(TK) - start[e_sorted])
    n_pad = ((TK + DISPATCH_BLOCK - 1) // DISPATCH_BLOCK + N_EXPERTS) * DISPATCH_BLOCK
    n_blocks = n_pad // DISPATCH_BLOCK
    x_buf = jnp.zeros((n_pad, D), xt.dtype).at[dest].set(xt[tok_sorted])
    block_e = jnp.clip(jnp.searchsorted(pend, jnp.arange(n_blocks) * DISPATCH_BLOCK, side='right'),
                       0, N_EXPERTS - 1)

    def expert_block(args):
        xb, e = args
        hid = jax.nn.silu(xb @ w_e_gate[e]) * (xb @ w_e_up[e])
        return hid @ w_e_down[e]

    y_buf = lax.map(expert_block, (x_buf.reshape(n_blocks, DISPATCH_BLOCK, D), block_e))
    y = y_buf.reshape(n_pad, D)[dest] * w_sorted[:, None].astype(xt.dtype)
    out = jax.ops.segment_sum(y, tok_sorted, num_segments=T)
    return out.reshape(B, S, D)


def setup_inputs(seed: int = 0) -> dict:
    key = jax.random.key(seed)
    ks = jax.random.split(key, 24)
    f32 = jnp.float32

    def nrm(k, shape, scale):
        return jax.random.normal(k, shape, f32) * scale

    a_base = jax.random.uniform(ks[9], (DEPTH, REC_WIDTH), f32, 0.9, 0.999)
    s = a_base ** (1.0 / LRU_C)
    lru_lambda = jnp.log(s) - jnp.log1p(-s)
    return {
        'x': nrm(ks[0], (BATCH, SEQ, D_MODEL), 1.0),
        'norm1_g': 1.0 + nrm(ks[1], (DEPTH, D_MODEL), 0.02),
        'w_in': nrm(ks[2], (DEPTH, D_MODEL, IN_WIDTH), D_MODEL ** -0.5),
        'conv_w': nrm(ks[3], (DEPTH, CONV_WIDTH, REC_WIDTH), CONV_WIDTH ** -0.5),
        'conv_b': nrm(ks[4], (DEPTH, REC_WIDTH), 0.02),
        'w_rg_a': nrm(ks[5], (DEPTH, REC_HEADS, REC_HEAD_DIM, REC_HEAD_DIM), REC_HEAD_DIM ** -0.5),
        'b_rg_a': nrm(ks[6], (DEPTH, REC_HEADS, REC_HEAD_DIM), 0.02),
        'w_rg_x': nrm(ks[7], (DEPTH, REC_HEADS, REC_HEAD_DIM, REC_HEAD_DIM), REC_HEAD_DIM ** -0.5),
        'b_rg_x': nrm(ks[8], (DEPTH, REC_HEADS, REC_HEAD_DIM), 0.02),
        'lru_lambda': lru_lambda,
        'rel_bias': nrm(ks[10], (DEPTH, ATT_HEADS, 2 * MAX_REL + 1), 0.2),
        'g_rec_out': 1.0 + nrm(ks[11], (DEPTH, REC_WIDTH), 0.02),
        'g_att_out': 1.0 + nrm(ks[12], (DEPTH, ATT_WIDTH), 0.02),
        'w_out': nrm(ks[13], (DEPTH, MIX_WIDTH, D_MODEL), MIX_WIDTH ** -0.5),
        'norm2_g': 1.0 + nrm(ks[14], (DEPTH, D_MODEL), 0.02),
        'w_group': nrm(ks[15], (DEPTH, D_MODEL, N_GROUPS), D_MODEL ** -0.5),
        'b_group': nrm(ks[16], (DEPTH, N_GROUPS), 0.01),
        'w_router': nrm(ks[17], (DEPTH, D_MODEL, N_EXPERTS), D_MODEL ** -0.5),
        'b_router': nrm(ks[18], (DEPTH, N_EXPERTS), 0.01),
        'w_e_gate': nrm(ks[19], (DEPTH, N_EXPERTS, D_MODEL, D_FF_EXPERT), D_MODEL ** -0.5),
        'w_e_up': nrm(ks[20], (DEPTH, N_EXPERTS, D_MODEL, D_FF_EXPERT), D_MODEL ** -0.5),
        'w_e_down': nrm(ks[21], (DEPTH, N_EXPERTS, D_FF_EXPERT, D_MODEL), D_FF_EXPERT ** -0.5),
        'final_g': 1.0 + nrm(ks[22], (D_MODEL,), 0.02),
    }


def reference(x, norm1_g, w_in, conv_w, conv_b, w_rg_a, b_rg_a, w_rg_x, b_rg_x, lru_lambda,
              rel_bias, g_rec_out, g_att_out, w_out, norm2_g, w_group, b_group, w_router,
              b_router, w_e_gate, w_e_up, w_e_down, final_g):
    splits = [REC_WIDTH, 2 * REC_WIDTH, 2 * REC_WIDTH + ATT_WIDTH, 2 * REC_WIDTH + 2 * ATT_WIDTH]
    h = x
    for l in range(DEPTH):
        xn = rms_norm(h, norm1_g[l])
        z = xn @ w_in[l]
        u, gate, q, k, v = jnp.split(z, splits, axis=-1)
        y_rec = rg_lru_group(u, gate, conv_w[l], conv_b[l], w_rg_a[l], b_rg_a[l],
                             w_rg_x[l], b_rg_x[l], lru_lambda[l])
        y_att = chunk_attention_group(q, k, v, rel_bias[l])
        mix = jnp.concatenate([rms_norm(y_rec, g_rec_out[l]), rms_norm(y_att, g_att_out[l])], axis=-1)
        h = h + mix @ w_out[l]
        h = h + hierarchical_moe(rms_norm(h, norm2_g[l]), w_group[l], b_group[l], w_router[l],
                                 b_router[l], w_e_gate[l], w_e_up[l], w_e_down[l])
    return rms_norm(h, final_g)
```

```python
from contextlib import ExitStack
import numpy as np
import concourse.bass as bass
import concourse.mybir as mybir
from concourse.bass_utils import run_bass_kernel_spmd

F32 = mybir.dt.float32
BF16 = mybir.dt.bfloat16
AF = mybir.ActivationFunctionType
ALU = mybir.AluOpType
AX = mybir.AxisListType

ENGS = ("pe", "act", "dve", "pool", "sp")
T = 2048
D = 1024
NT = 16
EPS = 1e-6
NEXP = 16


class Ev:
    __slots__ = ("kind", "eng", "idx", "sem", "val")

    def __init__(self, kind, eng=None, idx=None, sem=None, val=None):
        self.kind, self.eng, self.idx, self.sem, self.val = kind, eng, idx, sem, val


class Prog:
    def __init__(self, nc, n_dma_sems=16, same_engine_sync=True):
        self.nc = nc
        self.ops = {e: [] for e in ENGS}
        self.same_engine_sync = same_engine_sync
        self.n_dma_sems = n_dma_sems
        self.dma_sem_tot = [0] * n_dma_sems
        self.dma_rr = 0
        self.dma_rr2 = [0, 0]
        self.last_w = {}
        self.readers = {}

    def _deps_for(self, reads, writes):
        deps = []
        for k in reads:
            if k in self.last_w:
                deps.append(self.last_w[k])
        for k in writes:
            if k in self.last_w:
                deps.append(self.last_w[k])
            deps.extend(self.readers.get(k, ()))
        return deps

    def _commit(self, ev, reads, writes):
        for k in reads:
            self.readers.setdefault(k, []).append(ev)
        for k in writes:
            self.last_w[k] = ev
            self.readers[k] = []

    def op(self, eng, fn, reads=(), writes=(), inc=True, extra=()):
        deps = self._deps_for(reads, writes) + list(extra)
        idx = len(self.ops[eng])
        self.ops[eng].append(dict(fn=fn, deps=deps, inc=inc, dma=None))
        ev = Ev("eng", eng=eng, idx=idx)
        self._commit(ev, reads, writes)
        return ev

    def dma(self, eng, fn, reads=(), writes=(), extra=()):
        deps = self._deps_for(reads, writes) + list(extra)
        half = self.n_dma_sems // 2
        r = 1 if eng == "pool" else 0
        s = r * half + self.dma_rr2[r]
        self.dma_rr2[r] = (self.dma_rr2[r] + 1) % half
        prev = self.dma_sem_tot[s]
        if prev:
            deps.append(Ev("dma", sem=s, val=prev))
        self.dma_sem_tot[s] = prev + 16
        ev = Ev("dma", sem=s, val=prev + 16)
        self.ops[eng].append(dict(fn=fn, deps=deps, inc=False, dma=s))
        self._commit(ev, reads, writes)
        return ev

    def fence(self):
        evs = []
        for e in ENGS:
            ops = self.ops[e]
            j = len(ops) - 1
            while j >= 0 and (ops[j].get("nop") or ops[j]["dma"] is not None):
                j -= 1
            if j >= 0:
                assert ops[j]["inc"], "fence: last op on %s has no inc" % e
                evs.append(Ev("eng", eng=e, idx=j))
        for s in range(self.n_dma_sems):
            if self.dma_sem_tot[s]:
                evs.append(Ev("dma", sem=s, val=self.dma_sem_tot[s]))
        for e in ENGS:
            self.ops[e].append(dict(fn=None, deps=list(evs), inc=False, dma=None, nop=True))

    def emit(self, final_waits=()):
        nc = self.nc
        with ExitStack() as st:
            esem = {e: st.enter_context(nc.semaphore("c_" + e)) for e in ENGS}
            dsem = [st.enter_context(nc.semaphore("d%d" % i)) for i in range(self.n_dma_sems)]
            cum = {}
            for e in ENGS:
                c = 0
                arr = []
                for o in self.ops[e]:
                    if o["inc"]:
                        c += 1
                    arr.append(c)
                cum[e] = arr
            ninc = {e: (cum[e][-1] if cum[e] else 0) for e in ENGS}

            def resolve(ev):
                if ev.kind == "dma":
                    return dsem[ev.sem], ev.val
                o = self.ops[ev.eng][ev.idx]
                v = cum[ev.eng][ev.idx]
                if not o["inc"]:
                    v += 1
                    assert v <= ninc[ev.eng], "event on non-inc op without later inc"
                return esem[ev.eng], v

            block = st.enter_context(nc.Block())

            def make(ename):
                ops = self.ops[ename]

                def body(eng):
                    waited = {}
                    for o in ops:
                        need = {}
                        for ev in o["deps"]:
                            if ev.kind == "eng" and ev.eng == ename and not o.get("nop") and (
                                    ename == "pe" or not self.same_engine_sync):
                                continue
                            sem, v = resolve(ev)
                            key = id(sem)
                            if v > need.get(key, (None, 0))[1]:
                                need[key] = (sem, v)
                        for key, (sem, v) in need.items():
                            if waited.get(key, 0) >= v:
                                continue
                            eng.wait_ge(sem, v)
                            waited[key] = v
                        if o.get("nop"):
                            continue
                        ins = o["fn"](eng)
                        if o["dma"] is not None:
                            ins.then_inc(dsem[o["dma"]], 16)
                        elif o["inc"]:
                            ins.then_inc(esem[ename], 1)
                    if ename == "sp":
                        for ev in final_waits:
                            sem, v = resolve(ev)
                            eng.wait_ge(sem, v)
                return body

            block.tensor(make("pe"))
            block.scalar(make("act"))
            block.vector(make("dve"))
            block.gpsimd(make("pool"))
            block.sync(make("sp"))


SB_LO = 16512
SB_HI = 229344
PERSIST = 8192
A0 = SB_LO + PERSIST


def build_nc(dbg=False, upto=99):
    nc = bass.Bass("TRN2", target_bir_lowering=False)

    def din(name, shape, dt=F32):
        return nc.dram_tensor(name, list(shape), dt, kind="ExternalInput").ap()

    x_d = din("x", [T, D])
    win_d = din("w_in", [D, 2560])
    wout_d = din("w_out", [D, D])
    weg_d = din("weg", [NEXP, D, 512])
    weu_d = din("weu", [NEXP, D, 512])
    wed_d = din("wed", [NEXP, 512, D])
    g1_d = din("g1bc", [128, D])
    g2_d = din("g2bc", [128, D])
    gf_d = din("gfbc", [128, D])
    gatt_d = din("gattbc", [128, 512])
    cols_d = din("cols", [128, 64])
    bd_d = din("bd", [128, 1024])
    braw_d = din("braw", [128, 2048])
    wr_d = din("wr", [D, 20])
    brbc_d = din("brbc", [128, 20])
    id_d = din("identf", [128, 128])
    ec_d = din("ecc", [128, 256])
    ut_d = din("utri", [128, 128])
    out_d = nc.dram_tensor("out", [T, D], F32, kind="ExternalOutput").ap()
    dbg_d = {}

    def dout(name, shape):
        dbg_d[name] = nc.dram_tensor(name, list(shape), F32, kind="ExternalOutput").ap()
        return dbg_d[name]

    def sb_at(name, shape, dt, off):
        nbytes = int(np.prod(shape[1:])) * (2 if dt == BF16 else 4)
        assert off % 32 == 0, (name, off)
        assert SB_LO <= off and off + nbytes <= SB_HI, (name, off, nbytes)
        return nc.alloc_sbuf_tensor_at(name, list(shape), dt, offset=off).ap()

    class Arena:
        def __init__(self, lo, hi):
            self.lo, self.hi, self.cur = lo, hi, lo

        def take(self, name, shape, dt):
            nbytes = int(np.prod(shape[1:])) * (2 if dt == BF16 else 4)
            nbytes = (nbytes + 31) // 32 * 32
            off = self.cur
            assert off + nbytes <= self.hi, ("arena overflow", name, off, nbytes, self.hi)
            self.cur += nbytes
            return sb_at(name, shape, dt, off)

    pers = Arena(SB_LO, A0)
    identf = pers.take("identf", [128, 128], F32)
    onesf = pers.take("onesf", [128, 128], F32)
    identb = pers.take("identb", [128, 128], BF16)
    cols = pers.take("colsb", [128, 64], F32)
    stat = pers.take("stat", [128, 16 * 12], F32)
    brbc = pers.take("brbc", [128, 20], F32)
    wr = pers.take("wr", [128, 8, 20], F32)
    logits = pers.take("logits", [128, NT, 20], F32)
    uhalo = pers.take("uhalo", [128, 4, 4], F32)
    hprev = pers.take("hprev", [128, 4], F32)
    scal = pers.take("scal", [128, 32], F32)

    CW = lambda c, j: cols[:, c * 4 + j: c * 4 + j + 1]
    CB = lambda c: cols[:, 16 + c: 17 + c]
    BA = lambda c: cols[:, 20 + c: 21 + c]
    BX = lambda c: cols[:, 24 + c: 25 + c]
    LAM = cols[:, 28:32]
    GREC = lambda c: cols[:, 32 + c: 33 + c]
    BFAR = lambda h: cols[:, 36 + h: 37 + h]
    CL = lambda c: scal[:, 8 + c: 9 + c]
    CL2 = lambda c: scal[:, 12 + c: 13 + c]

    def st(k, i):
        return stat[:, k * 16 + i: k * 16 + i + 1]

    xnT = sb_at("xnT", [128, 8, T], BF16, A0 + 0)
    mixT = sb_at("mixT", [128, 8, T], BF16, A0 + 32768)
    qT = sb_at("qT", [128, 4, T], BF16, A0 + 65536)
    kT = sb_at("kT", [128, 4, T], BF16, A0 + 81920)
    vaug = sb_at("vaug", [128, NT, 8, 65], BF16, A0 + 98304)
    A1LO = A0 + 114944

    pT = nc.alloc_psum_tensor("pT", [128, 1024], BF16).ap()
    pW = nc.alloc_psum_tensor("pW", [128, 1024], F32).ap()
    pS = [nc.alloc_psum_tensor("pS%d" % i, [128, 512], F32).ap() for i in range(4)]
    pT2 = nc.alloc_psum_tensor("pT2", [128, 1024], BF16).ap()
    W0, W1 = pW[:, 0:512], pW[:, 512:1024]

    P = Prog(nc)
    final_evs = []

    def dbg_store(name, ap_sb, key, shape, view=None):
        if not dbg:
            return
        d = dout(name, shape)
        keys = key if isinstance(key, list) else [key]
        final_evs.append(P.dma("pool", lambda e, d=d, a=ap_sb: e.dma_start(out=(d if view is None else view(d)), in_=a),
                               reads=keys))

    P.dma("sp", lambda e: e.dma_start(out=identf, in_=id_d), writes=["identf"])
    P.dma("sp", lambda e: e.dma_start(out=cols, in_=cols_d), writes=["cols"])
    P.dma("sp", lambda e: e.dma_start(out=brbc, in_=brbc_d), writes=["brbc"])
    P.dma("sp", lambda e: e.dma_start(out=wr, in_=wr_d.rearrange("(kc p) n -> p kc n", p=128)), writes=["wr"])
    P.dma("pool", lambda e: e.dma_start(out=identb, in_=id_d), writes=["identb"])
    P.op("pool", lambda e: e.memset(onesf, 1.0), writes=["onesf"])
    P.op("pool", lambda e: e.memset(stat, 0.0), writes=["stat0"])
    P.op("pool", lambda e: e.memset(uhalo, 0.0), writes=["uhalo"])
    P.op("pool", lambda e: e.memset(hprev, 0.0), writes=["hprev"])
    P.op("pool", lambda e: e.memset(vaug[:, :, :, 64:65], 1.0), writes=["vaug1"])
    P.op("act", lambda e: e.activation(out=scal[:, 0:4], in_=LAM, func=AF.Exp, scale=-1.0), reads=["cols"], writes=["scal"])
    P.op("act", lambda e: e.activation(out=scal[:, 4:8], in_=scal[:, 0:4], func=AF.Ln, bias=1.0), reads=["scal"], writes=["scal"])
    P.op("dve", lambda e: e.tensor_scalar(out=scal[:, 8:12], in0=scal[:, 4:8], scalar1=-8.0, scalar2=None, op0=ALU.mult), reads=["scal"], writes=["scal"])
    P.op("dve", lambda e: e.tensor_scalar(out=scal[:, 12:16], in0=scal[:, 4:8], scalar1=-16.0, scalar2=None, op0=ALU.mult), reads=["scal"], writes=["scal"])

    def rms_stats(src_ap, src_key, junk_ap, junk_key, k0, i, n):
        sk = ("stat", k0, i)
        P.op("act", lambda e: e.activation(out=junk_ap, in_=src_ap, func=AF.Square, accum_out=st(k0, i)),
             reads=[src_key, "stat0"], writes=[junk_key, sk])
        P.op("act", lambda e: e.activation(out=st(k0 + 1, i), in_=st(k0, i), func=AF.Ln, scale=1.0 / n, bias=EPS),
             reads=[sk], writes=[sk])
        P.op("act", lambda e: e.activation(out=st(k0 + 2, i), in_=st(k0 + 1, i), func=AF.Exp, scale=-0.5),
             reads=[sk], writes=[sk])

    a1 = Arena(A1LO, SB_HI)
    wring = [a1.take("wring%d" % i, [128, 8, 128], BF16) for i in range(2)]
    wug = a1.take("wug", [128, 8, 8, 128], BF16)
    junk = a1.take("junk", [128, D], BF16)
    bd = a1.take("bd", [128, 1024], F32)
    Ub = a1.take("Ub", [128, 4, 516], F32)
    Gt = a1.take("Gt", [128, 4, 512], F32)
    UC = a1.take("UC", [128, 4, 512], F32)
    RR = a1.take("RR", [128, 4, 512], F32)
    IG = a1.take("IG", [128, 4, 512], F32)
    A2 = a1.take("A2", [128, 4, 512], F32)
    GT = a1.take("GT", [128, 4, 512], F32)
    rb = a1.take("rb", [128, 512], F32)
    n1 = Arena(A0 + 32768 + 16384, A0 + 65536)
    xstage = [n1.take("xstage%d" % i, [128, D], F32) for i in range(2)]
    xs = [n1.take("xs%d" % i, [128, D], BF16) for i in range(2)]
    g1bc = n1.take("g1bc", [128, D], F32)

    P.dma("sp", lambda e: e.dma_start(out=g1bc, in_=g1_d), writes=["g1bc"])
    P.dma("sp", lambda e: e.dma_start(out=bd, in_=bd_d), writes=["bd"])

    win_v = win_d.rearrange("(kc p) n -> p kc n", p=128)
    xnT_all = [("xnT", i) for i in range(NT)]

    def norm1_tile(i):
        xst = xstage[i % 2]
        kx = "xstage%d" % (i % 2)
        kxs = "xs%d" % (i % 2)
        P.dma("sp", lambda e: e.dma_start(out=xst, in_=x_d[i * 128:(i + 1) * 128, :]), writes=[kx])
        rms_stats(xst, kx, junk, "junk", 0, i, D)
        P.op("dve", lambda e: e.scalar_tensor_tensor(out=xs[i % 2], in0=xst, scalar=st(2, i), in1=g1bc, op0=ALU.mult, op1=ALU.mult),
             reads=[kx, ("stat", 0, i), "g1bc"], writes=[kxs])
        tb_, tk_ = (pT, "pT") if i % 2 == 0 else (pT2, "pT2")
        for c in range(8):
            P.op("pe", lambda e, c=c: e.transpose(tb_[:, c * 128:(c + 1) * 128], xs[i % 2][:, c * 128:(c + 1) * 128], identb),
                 reads=[kxs, "identb"], writes=[tk_], inc=(c == 7))
        P.op("act", lambda e: e.copy(out=xnT[:, :, i * 128:(i + 1) * 128], in_=tb_.rearrange("p (c t) -> p c t", c=8)),
             reads=[tk_], writes=[("xnT", i)])

    mm_banks = [W0, W1]
    mm_keys = ["W0", "W1"]
    mmc = [0]
    evc = [0]

    def next_bank():
        b = mmc[0] % 2
        mmc[0] += 1
        return mm_banks[b], mm_keys[b]

    def evac_copy(out_ap, in_ap, reads, writes, eng=None):
        if eng is None:
            eng = "act" if evc[0] % 2 == 0 else "dve"
            evc[0] += 1
        if eng == "act":
            P.op("act", lambda e: e.copy(out=out_ap, in_=in_ap), reads=reads, writes=writes)
        else:
            P.op("dve", lambda e: e.tensor_copy(out=out_ap, in_=in_ap), reads=reads, writes=writes)

    nring = [0]

    def qkv_chunk(j, tb):
        slot = j % 2
        wk = "wring%d" % slot
        if j < 8:
            dstT, dkey = (qT, "qT") if j < 4 else (kT, "kT")
            bank, bkey = next_bank()
            for kc in range(8):
                P.op("pe", lambda e, kc=kc: e.matmul(bank, wring[slot][:, kc, :], xnT[:, kc, tb * 512:(tb + 1) * 512], start=(kc == 0), stop=(kc == 7)),
                     reads=[wk] + xnT_all[tb * 4:(tb + 1) * 4], writes=[bkey], inc=(kc == 7))
            evac_copy(dstT[:, j % 4, tb * 512:(tb + 1) * 512], bank, [bkey], [(dkey, j % 4, tb)], eng="act")
        else:
            jj = j - 8
            bank, bkey = next_bank()
            for tt in range(4):
                i = tb * 4 + tt
                for kc in range(8):
                    P.op("pe", lambda e, kc=kc, i=i, tt=tt: e.matmul(bank[:, tt * 128:(tt + 1) * 128], xnT[:, kc, i * 128:(i + 1) * 128], wring[slot][:, kc, :],
                                                                    start=(kc == 0), stop=(kc == 7)),
                         reads=[wk, ("xnT", i)], writes=[bkey], inc=(kc == 7))
            evac_copy(vaug[:, tb * 4:(tb + 1) * 4, 2 * jj:2 * jj + 2, 0:64], bank.rearrange("p (t h d) -> p t h d", t=4, h=2),
                      [bkey], [("vaug", jj, tb)], eng="act")

    def load_wchunk(j):
        slot = j % 2
        col0 = 1024 + j * 128
        P.dma("pool", lambda e: e.dma_start(out=wring[slot], in_=win_v[:, :, col0:col0 + 128]), writes=["wring%d" % slot])

    for cc in range(8):
        P.dma("pool", lambda e, cc=cc: e.dma_start(out=wug[:, cc, :, :], in_=win_v[:, :, cc * 128:(cc + 1) * 128]), writes=[("wug", cc)])

    Gbanks = [(pS[0], "pS0", pS[1], "pS1"), (pS[2], "pS2", pS[3], "pS3")]
    gbn = [0]
    FL = lambda t: t.rearrange("p c t -> p (c t)")

    for i in range(4):
        norm1_tile(i)
    for tb in range(4):
        tsl = slice(tb * 512, (tb + 1) * 512)
        for c in range(4):
            for which in range(2):
                bank, bkey = next_bank()
                cc = which * 4 + c
                for kc in range(8):
                    P.op("pe", lambda e, bank=bank, cc=cc, kc=kc, tsl=tsl: e.matmul(bank, wug[:, cc, kc, :], xnT[:, kc, tsl], start=(kc == 0), stop=(kc == 7)),
                         reads=[("wug", cc)] + xnT_all[tb * 4:(tb + 1) * 4], writes=[bkey], inc=(kc == 7))
                if which == 0:
                    P.op("pool", lambda e, c=c: e.tensor_copy(out=Ub[:, c, 1:4], in_=uhalo[:, c, 1:4]), reads=["uhalo"], writes=[("Ub", c)])
                    P.op("act", lambda e, bank=bank, c=c: e.copy(out=Ub[:, c, 4:516], in_=bank), reads=[bkey, ("Ub", c)], writes=[("Ub", c)])
                    P.op("pool", lambda e, c=c: e.tensor_copy(out=uhalo[:, c, 1:4], in_=Ub[:, c, 513:516]), reads=[("Ub", c)], writes=["uhalo"])
                else:
                    P.op("dve", lambda e, bank=bank, c=c: e.tensor_copy(out=Gt[:, c, :], in_=bank), reads=[bkey], writes=[("Gt", c)])
        if tb + 1 < 4:
            for i in range(4 * (tb + 1), 4 * (tb + 2)):
                norm1_tile(i)
        for c in range(4):
            P.op("pool", lambda e, c=c: e.tensor_tensor(out=GT[:, c, :], in0=Gt[:, c, :], in1=Gt[:, c, :], op=ALU.mult), reads=[("Gt", c)], writes=[("GT", c)])
            P.op("pool", lambda e, c=c: e.tensor_scalar(out=GT[:, c, :], in0=GT[:, c, :], scalar1=0.044715, scalar2=1.0, op0=ALU.mult, op1=ALU.add),
                 reads=[("GT", c)], writes=[("GT", c)])
            P.op("pool", lambda e, c=c: e.tensor_tensor(out=GT[:, c, :], in0=GT[:, c, :], in1=Gt[:, c, :], op=ALU.mult), reads=[("GT", c), ("Gt", c)], writes=[("GT", c)])
        for c in range(4):
            P.op("dve", lambda e, c=c: e.tensor_scalar(out=UC[:, c, :], in0=Ub[:, c, 4:516], scalar1=CW(c, 3), scalar2=CB(c), op0=ALU.mult, op1=ALU.add),
                 reads=[("Ub", c), "cols"], writes=[("UC", c)])
            for k in (1, 2, 3):
                P.op("dve", lambda e, c=c, k=k: e.scalar_tensor_tensor(out=UC[:, c, :], in0=Ub[:, c, 4 - k:516 - k], scalar=CW(c, 3 - k), in1=UC[:, c, :],
                                                                       op0=ALU.mult, op1=ALU.add), reads=[("Ub", c), "cols", ("UC", c)], writes=[("UC", c)])
        for j in range(0, 4):
            load_wchunk(j)
            qkv_chunk(j, tb)
        for c in range(4):
            pr, kr, pi_, ki = Gbanks[gbn[0] % 2]
            gbn[0] += 1
            P.op("pe", lambda e, c=c, pr=pr: e.matmul(pr, bd[:, c * 128:(c + 1) * 128], UC[:, c, :], start=True, stop=True), reads=["bd", ("UC", c)], writes=[kr])
            P.op("pe", lambda e, c=c, pi_=pi_: e.matmul(pi_, bd[:, 512 + c * 128:512 + (c + 1) * 128], UC[:, c, :], start=True, stop=True),
                 reads=["bd", ("UC", c)], writes=[ki])
            P.op("act", lambda e, c=c, pr=pr: e.activation(out=RR[:, c, :], in_=pr, func=AF.Sigmoid, bias=BA(c)), reads=[kr, "cols"], writes=[("RR", c)])
            P.op("act", lambda e, c=c, pi_=pi_: e.activation(out=IG[:, c, :], in_=pi_, func=AF.Sigmoid, bias=BX(c)), reads=[ki, "cols"], writes=[("IG", c)])
        for c in range(4):
            P.op("act", lambda e, c=c: e.activation(out=GT[:, c, :], in_=GT[:, c, :], func=AF.Sigmoid, scale=1.5957691216057308), reads=[("GT", c)], writes=[("GT", c)])
        for j in range(4, 8):
            load_wchunk(j)
            qkv_chunk(j, tb)
        for c in range(4):
            P.op("act", lambda e, c=c: e.activation(out=A2[:, c, :], in_=RR[:, c, :], func=AF.Exp, scale=CL2(c)), reads=[("RR", c), "scal"], writes=[("A2", c)])
            P.op("act", lambda e, c=c: e.activation(out=RR[:, c, :], in_=RR[:, c, :], func=AF.Exp, scale=CL(c)), reads=[("RR", c), "scal"], writes=[("RR", c)])
        allk = lambda n: [(n, c) for c in range(4)]
        P.op("dve", lambda e: e.tensor_scalar(out=FL(A2), in0=FL(A2), scalar1=-1.0, scalar2=1.0, op0=ALU.mult, op1=ALU.add), reads=allk("A2"), writes=allk("A2"))
        P.op("act", lambda e: e.activation(out=FL(A2), in_=FL(A2), func=AF.Ln), reads=allk("A2"), writes=allk("A2"))
        P.op("act", lambda e: e.activation(out=FL(A2), in_=FL(A2), func=AF.Exp, scale=0.5), reads=allk("A2"), writes=allk("A2"))
        P.op("dve", lambda e: e.tensor_tensor(out=FL(IG), in0=FL(IG), in1=FL(UC), op=ALU.mult), reads=allk("IG") + allk("UC"), writes=allk("IG"))
        P.op("dve", lambda e: e.tensor_tensor(out=FL(IG), in0=FL(IG), in1=FL(A2), op=ALU.mult), reads=allk("IG") + allk("A2"), writes=allk("IG"))
        for c in range(4):
            P.op("dve", lambda e, c=c: e.tensor_tensor_scan(out=A2[:, c, :], data0=RR[:, c, :], data1=IG[:, c, :], initial=hprev[:, c:c + 1],
                                                            op0=ALU.mult, op1=ALU.add), reads=[("RR", c), ("IG", c), "hprev", ("A2", c)], writes=[("A2", c)])
            P.op("pool", lambda e, c=c: e.tensor_copy(out=hprev[:, c:c + 1], in_=A2[:, c, 511:512]), reads=[("A2", c)], writes=["hprev"])
        for j in range(8, 12):
            load_wchunk(j)
            qkv_chunk(j, tb)
        P.op("dve", lambda e: e.tensor_tensor(out=FL(GT), in0=FL(GT), in1=FL(Gt), op=ALU.mult), reads=allk("GT") + allk("Gt"), writes=allk("GT"))
        P.op("dve", lambda e: e.tensor_tensor(out=FL(GT), in0=FL(GT), in1=FL(A2), op=ALU.mult), reads=allk("GT") + allk("A2"), writes=allk("GT"))
        P.op("act", lambda e: e.activation(out=FL(UC), in_=FL(GT), func=AF.Square), reads=allk("GT") + allk("UC"), writes=allk("UC"))
        pr, kr, _, _ = Gbanks[gbn[0] % 2]
        gbn[0] += 1
        for c in range(4):
            P.op("pe", lambda e, c=c, pr=pr: e.matmul(pr, onesf, UC[:, c, :], start=(c == 0), stop=(c == 3)), reads=["onesf", ("UC", c)], writes=[kr], inc=(c == 3))
        P.op("act", lambda e, pr=pr: e.activation(out=rb, in_=pr, func=AF.Ln, scale=1.0 / 512, bias=EPS), reads=[kr], writes=["rb"])
        P.op("act", lambda e: e.activation(out=rb, in_=rb, func=AF.Exp, scale=-0.5), reads=["rb"], writes=["rb"])
        for c in range(4):
            P.op("dve", lambda e, c=c, tsl=tsl: e.scalar_tensor_tensor(out=mixT[:, c, tsl], in0=GT[:, c, :], scalar=GREC(c), in1=rb, op0=ALU.mult, op1=ALU.mult),
                 reads=[("GT", c), "rb", "cols"], writes=[("mixT", c, tb)])
    if dbg:
        dbg_store("dbg_xnT", xnT, xnT_all, [128, 8 * T], view=lambda d: d.rearrange("p (c t) -> p c t", c=8))
        dbg_store("dbg_mixrec", mixT[:, 0:4, :], [("mixT", c, tb) for c in range(4) for tb in range(4)], [128, 4 * T], view=lambda d: d.rearrange("p (c t) -> p c t", c=4))
        dbg_store("dbg_qT", qT, [("qT", c, tb) for c in range(4) for tb in range(4)], [128, 4 * T], view=lambda d: d.rearrange("p (c t) -> p c t", c=4))
        dbg_store("dbg_kT", kT, [("kT", c, tb) for c in range(4) for tb in range(4)], [128, 4 * T], view=lambda d: d.rearrange("p (c t) -> p c t", c=4))
        dbg_store("dbg_v", vaug, [("vaug", c, tb) for c in range(4) for tb in range(4)] + ["vaug1"], [128, NT * 8 * 65], view=lambda d: d.rearrange("p (t h d) -> p t h d", t=NT, h=8))

    P.fence()
    if upto <= 1:
        return finish(nc, P, final_evs, out_d, dbg_d)

    CAP = 512
    NB = CAP // 128
    RROWS = NEXP * CAP + 128
    Xs_d = nc.dram_tensor("Xs_scr", [RROWS, D], BF16).ap()
    Ys_d = nc.dram_tensor("Ys_scr", [RROWS, D], F32).ap()
    a2r = Arena(A0, A0 + 32768)
    Eb = [a2r.take("E%d" % i, [128, 640], BF16) for i in range(2)]
    yatt = [a2r.take("yatt%d" % i, [128, 512], F32) for i in range(2)]
    yan = [a2r.take("yan%d" % i, [128, 512], BF16) for i in range(2)]
    braw = a2r.take("braw", [128, 8, 256], F32)
    bias8 = a2r.take("bias8", [128, 8, 256], BF16)
    gattbc = a2r.take("gattbc", [128, 512], F32)
    junk2 = a2r.take("junk2", [128, 512], BF16)
    rden = a2r.take("rden", [128, 16], F32)
    hbuf = sb_at("hbuf", [128, NT, D], F32, A1LO)
    wout = sb_at("wout", [128, 8, D], BF16, A1LO + 65536)

    zt = a2r.take("zt", [128, D], BF16)
    P.op("pool", lambda e: e.memset(zt, 0.0), writes=["zt"])
    P.dma("sp", lambda e: e.dma_start(out=braw, in_=braw_d.rearrange("p (h k) -> p h k", h=8)), writes=["braw"])
    P.dma("sp", lambda e: e.dma_start(out=gattbc, in_=gatt_d), writes=["gattbc"])
    P.op("dve", lambda e: e.tensor_scalar(out=bias8, in0=braw, scalar1=8.0, scalar2=None, op0=ALU.mult), reads=["braw"], writes=["bias8"])
    P.dma("pool", lambda e: e.dma_start(out=wout, in_=wout_d.rearrange("(kc p) n -> p kc n", p=128)), writes=["wout"])
    for i in range(NT):
        P.dma("sp", lambda e, i=i: e.dma_start(out=hbuf[:, i, :], in_=x_d[i * 128:(i + 1) * 128, :]), writes=[("hbuf", i)])
    for r in range(RROWS // 128):
        P.dma("sp", lambda e, r=r: e.dma_start(out=Xs_d[r * 128:(r + 1) * 128, :], in_=zt), reads=["zt"], writes=[("Xs0", r)])

    units = [(i, h) for i in range(NT) for h in range(8)]
    NU = len(units)
    Sbanks = [(pS[0], "pS0", pS[1], "pS1"), (pS[2], "pS2", pS[3], "pS3")]
    Obanks = [(W0, "W0"), (W1, "W1")]

    def tiles_of(i):
        return list(range(max(0, i - 4), i + 1))

    def S1(n):
        i, h = units[n]
        psA, kA, psB, kB = Sbanks[n % 2]
        pb = (h % 2) * 64
        qh = qT[pb:pb + 64, h // 2, i * 128:(i + 1) * 128]
        qkeys = [("qT", h // 2, i // 4)]
        lo = 0 if i >= 1 else 128
        P.op("pe", lambda e: e.matmul(psB[:, lo:256], identb, bias8[:, h, lo:256], start=True, stop=False),
             reads=["identb", "bias8"], writes=[kB], inc=False)
        near = [t for t in (i - 1, i) if t >= 0]
        for idx, t in enumerate(near):
            s = t - (i - 4)
            kh = kT[pb:pb + 64, h // 2, t * 128:(t + 1) * 128]
            last = (idx == len(near) - 1)
            P.op("pe", lambda e, s=s, kh=kh, last=last: e.matmul(psB[:, (s - 3) * 128:(s - 2) * 128], kh, qh, start=False, stop=last),
                 reads=qkeys + [("kT", h // 2, t // 4)], writes=[kB], inc=last)
        far = [t for t in (i - 4, i - 3, i - 2) if t >= 0]
        for idx, t in enumerate(far):
            s = t - (i - 4)
            kh = kT[pb:pb + 64, h // 2, t * 128:(t + 1) * 128]
            last = (idx == len(far) - 1)
            P.op("pe", lambda e, s=s, kh=kh: e.matmul(psA[:, s * 128:(s + 1) * 128], kh, qh, start=True, stop=True),
                 reads=qkeys + [("kT", h // 2, t // 4)], writes=[kA], inc=last)

    def S2(n):
        i, h = units[n]
        psA, kA, psB, kB = Sbanks[n % 2]
        E = Eb[n % 2]
        ek = "E%d" % (n % 2)
        far = [t for t in (i - 4, i - 3, i - 2) if t >= 0]
        if far:
            s0 = far[0] - (i - 4)
            P.op("act", lambda e: e.activation(out=E[:, s0 * 128:384], in_=psA[:, s0 * 128:384], func=AF.Exp, bias=BFAR(h), scale=0.125),
                 reads=[kA, "cols"], writes=[ek + "a"])
        lo = 0 if i >= 1 else 128
        P.op("act", lambda e: e.activation(out=E[:, 384 + lo:640], in_=psB[:, lo:256], func=AF.Exp, scale=0.125),
             reads=[kB], writes=[ek + "b"])

    def S3(n):
        i, h = units[n]
        E = Eb[n % 2]
        ek = "E%d" % (n % 2)
        po, pk = Obanks[n % 2]
        ts = tiles_of(i)
        first = True
        for t in ts:
            s = t - (i - 4)
            last = (t == i)
            ekey = ek + ("a" if s < 3 else "b")
            vkeys = [("vaug", h // 2, t // 4), "vaug1"]
            if s == 0:
                P.op("pe", lambda e, t=t: e.matmul(po[:, 0:65], E[64:128, 0:128], vaug[64:128, t, h, :], start=True, stop=False),
                     reads=[ekey] + vkeys, writes=[pk], inc=False)
                P.op("pe", lambda e, t=t: e.matmul(po[0:64, 0:65], E[0:64, 0:64], vaug[0:64, t, h, :], start=False, stop=False),
                     reads=[ekey] + vkeys, writes=[pk], inc=False)
                first = False
            else:
                P.op("pe", lambda e, t=t, s=s, first=first, last=last: e.matmul(po[:, 0:65], E[:, s * 128:(s + 1) * 128], vaug[:, t, h, :],
                                                                                 start=first, stop=last),
                     reads=[ekey] + vkeys, writes=[pk], inc=last)
                first = False

    def S4(n):
        i, h = units[n]
        po, pk = Obanks[n % 2]
        ya = yatt[i % 2]
        P.op("dve", lambda e: e.reciprocal(out=rden[:, h:h + 1], in_=po[:, 64:65]), reads=[pk], writes=["rden"])
        P.op("dve", lambda e: e.tensor_scalar(out=ya[:, h * 64:(h + 1) * 64], in0=po[:, 0:64], scalar1=rden[:, h:h + 1], scalar2=None, op0=ALU.mult),
             reads=[pk, "rden"], writes=[("yatt", i % 2)])
        if h == 7:
            yk = ("yatt", i % 2)

            def blk_norm(i=i, ya=ya, yk=yk):
                rms_stats(ya, yk, junk2, "junk2", 3, i, 512)
                P.op("dve", lambda e: e.scalar_tensor_tensor(out=yan[i % 2], in0=ya, scalar=st(5, i), in1=gattbc, op0=ALU.mult, op1=ALU.mult),
                     reads=[yk, ("stat", 3, i), "gattbc"], writes=[("yan", i % 2)])
                deferred.append([2, blk_end])

            def blk_end(i=i):
                for c in range(4):
                    P.op("pe", lambda e, c=c: e.transpose(pT[:, c * 128:(c + 1) * 128], yan[i % 2][:, c * 128:(c + 1) * 128], identb),
                         reads=[("yan", i % 2), "identb"], writes=["pT"], inc=(c == 3))
                P.op("act", lambda e: e.copy(out=mixT[:, 4:8, i * 128:(i + 1) * 128], in_=pT[:, 0:512].rearrange("p (c t) -> p c t", c=4)),
                     reads=["pT"], writes=[("mixTa", i)])
            deferred.append([2, blk_norm])

    deferred = []
    for n in range(NU + 1):
        if n < NU:
            S1(n)
        if n >= 1:
            S2(n - 1)
            S3(n - 1)
            S4(n - 1)
        for d_ in deferred:
            d_[0] -= 1
        while deferred and deferred[0][0] <= 0:
            deferred.pop(0)[1]()
    while deferred:
        deferred.pop(0)[1]()
    if dbg:
        dbg_store("dbg_mixatt", mixT[:, 4:8, :], [("mixTa", i) for i in range(NT)], [128, 4 * T], view=lambda d: d.rearrange("p (c t) -> p c t", c=4))
    if upto <= 2:
        P.fence()
        return finish(nc, P, final_evs, out_d, dbg_d)

    for i in range(NT):
        for half in range(2):
            bank, bkey = next_bank()
            for kc in range(8):
                rk = [("mixT", kc, i // 4)] if kc < 4 else [("mixTa", i)]
                P.op("pe", lambda e, bank=bank, kc=kc, i=i, half=half: e.matmul(bank, mixT[:, kc, i * 128:(i + 1) * 128], wout[:, kc, half * 512:(half + 1) * 512],
                                                                                 start=(kc == 0), stop=(kc == 7)),
                     reads=rk + ["wout"], writes=[bkey], inc=(kc == 7))
            P.op("dve", lambda e, bank=bank, i=i, half=half: e.tensor_tensor(out=hbuf[:, i, half * 512:(half + 1) * 512], in0=bank,
                                                                              in1=hbuf[:, i, half * 512:(half + 1) * 512], op=ALU.add),
                 reads=[bkey, ("hbuf", i)], writes=[("hbuf", i)])
    if dbg:
        dbg_store("dbg_h1", hbuf, [("hbuf", i) for i in range(NT)], [128, NT * D], view=lambda d: d.rearrange("p (t f) -> p t f", t=NT))
    P.fence()
    if upto <= 3:
        return finish(nc, P, final_evs, out_d, dbg_d)


    xtok = sb_at("xtok", [128, NT, D], BF16, A0)
    cr = Arena(A0 + 32768, A1LO)
    wg = [cr.take("wg%d" % i, [128, 8, 512], BF16) for i in range(2)]
    wu = [cr.take("wu%d" % i, [128, 8, 512], BF16) for i in range(2)]
    wd = [cr.take("wd%d" % i, [128, 4, D], BF16) for i in range(2)]
    R2 = cr.cur
    c1 = Arena(R2, A1LO)
    xs2 = c1.take("xs2", [128, D], F32)
    xn2f = c1.take("xn2f", [128, 8, 128], F32)
    g2bc = c1.take("g2bc", [128, D], F32)
    c2 = Arena(A1LO + 65536, SB_HI)
    junk3 = c2.take("junk3", [128, D], BF16)
    gfbc = c2.take("gfbc", [128, D], F32)
    yst = [c2.take("yst%d" % i, [128, D], F32) for i in range(2)]
    RT = [c2.take("rt%d" % i, [128, 16, 16], F32) for i in range(6)]
    rs_ = [c2.take("rs%d" % i, [128, 16, 4], F32) for i in range(3)]
    rc = c2.take("rc", [128, 16 * 8], F32)
    ECc = c2.take("ECc", [128, 16, 16], F32)
    Mb = c2.take("Mb", [128, 256], BF16)
    UTb = c2.take("UTb", [128, 128], BF16)
    onesb = c2.take("onesb", [128, 128], BF16)
    offf = c2.take("offf", [128, 32], F32)
    offi = c2.take("offi", [128, 32], mybir.dt.int32)
    ones16 = pers.take("ones16", [128, 256], F32)

    P.dma("sp", lambda e: e.dma_start(out=g2bc, in_=g2_d), writes=["g2bc"])
    P.dma("sp", lambda e: e.dma_start(out=gfbc, in_=gf_d), writes=["gfbc"])
    P.dma("sp", lambda e: e.dma_start(out=ECc, in_=ec_d.rearrange("p (t e) -> p t e", t=16)), writes=["ECc"])
    P.dma("pool", lambda e: e.dma_start(out=UTb, in_=ut_d), writes=["UTb"])
    P.op("pool", lambda e: e.memset(onesb, 1.0), writes=["onesb"])
    P.op("pool", lambda e: e.memset(ones16, 1.0), writes=["ones16"])
    P.op("pool", lambda e: e.memset(yst[0], 0.0), writes=[("yst", 0)])
    P.dma("sp", lambda e: e.dma_start(out=Ys_d[NEXP * CAP:RROWS, :], in_=yst[0]), reads=[("yst", 0)], writes=["Ysdummy"])

    def load_expert(e_):
        s = e_ % 2
        P.dma("pool", lambda e: e.dma_start(out=wg[s], in_=weg_d[e_].rearrange("(kc p) n -> p kc n", p=128)), writes=[("wg", s)])
        P.dma("pool", lambda e: e.dma_start(out=wu[s], in_=weu_d[e_].rearrange("(kc p) n -> p kc n", p=128)), writes=[("wu", s)])
        P.dma("pool", lambda e: e.dma_start(out=wd[s], in_=wed_d[e_].rearrange("(kc p) n -> p kc n", p=128)), writes=[("wd", s)])

    if upto > 3.5:
        load_expert(0)
        load_expert(1)

    def c1_front(i):
        hk = ("hbuf", i)
        P.op("dve", lambda e: e.scalar_tensor_tensor(out=xs2, in0=hbuf[:, i, :], scalar=st(8, i), in1=g2bc, op0=ALU.mult, op1=ALU.mult),
             reads=[hk, ("stat", 6, i), "g2bc"], writes=["xs2"])
        P.op("pool", lambda e: e.tensor_copy(out=xtok[:, i, :], in_=xs2), reads=["xs2"], writes=[("xtok", i)])
        for c in range(8):
            P.op("pe", lambda e, c=c: e.transpose(pW[:, c * 128:(c + 1) * 128], xs2[:, c * 128:(c + 1) * 128], identf),
                 reads=["xs2", "identf"], writes=["W0", "W1"], inc=(c == 7))

    def c1_back(i):
        for hb, (bk, bkk) in enumerate(((W0, "W0"), (W1, "W1"))):
            P.op("act", lambda e, hb=hb, bk=bk: e.copy(out=xn2f[:, hb * 4:(hb + 1) * 4, :], in_=bk.rearrange("p (c t) -> p c t", c=4)),
                 reads=[bkk], writes=[("xn2f", hb)])
        for kc in range(8):
            P.op("pe", lambda e, kc=kc: e.matmul(pS[0][:, 0:20], xn2f[:, kc, :], wr[:, kc, :], start=(kc == 0), stop=(kc == 7)),
                 reads=[("xn2f", 0), ("xn2f", 1), "wr"], writes=["pS0"], inc=(kc == 7))
        P.op("dve", lambda e: e.tensor_tensor(out=logits[:, i, :], in0=pS[0][:, 0:20], in1=brbc, op=ALU.add),
             reads=["pS0", "brbc"], writes=["logits"])

    rms_stats(hbuf[:, 0, :], ("hbuf", 0), junk3, "junk3", 6, 0, D)
    rms_stats(hbuf[:, 1, :], ("hbuf", 1), junk3, "junk3", 6, 1, D)
    c1_front(0)
    for i in range(NT):
        if i + 2 < NT:
            rms_stats(hbuf[:, i + 2, :], ("hbuf", i + 2), junk3, "junk3", 6, i + 2, D)
        for hb, (bk, bkk) in enumerate(((W0, "W0"), (W1, "W1"))):
            P.op("act", lambda e, hb=hb, bk=bk: e.copy(out=xn2f[:, hb * 4:(hb + 1) * 4, :], in_=bk.rearrange("p (c t) -> p c t", c=4)),
                 reads=[bkk], writes=[("xn2f", hb)])
        for kc in range(8):
            P.op("pe", lambda e, kc=kc: e.matmul(pS[0][:, 0:20], xn2f[:, kc, :], wr[:, kc, :], start=(kc == 0), stop=(kc == 7)),
                 reads=[("xn2f", 0), ("xn2f", 1), "wr"], writes=["pS0"], inc=(kc == 7))
        if i + 1 < NT:
            c1_front(i + 1)
        P.op("dve", lambda e, i=i: e.tensor_tensor(out=logits[:, i, :], in0=pS[0][:, 0:20], in1=brbc, op=ALU.add),
             reads=["pS0", "brbc"], writes=["logits"])
    if upto <= 3.2:
        dbg_store("dbg_logits", logits, "logits", [128, NT * 20], view=lambda d: d.rearrange("p (t f) -> p t f", t=NT))
        P.fence()
        return finish(nc, P, final_evs, out_d, dbg_d)

    LG = logits[:, :, 0:4]
    LE = logits[:, :, 4:20]
    mg = rc[:, 0:16]
    sg_ = rc[:, 16:32]
    pg = rc[:, 32:48]
    m1 = rc[:, 48:64]
    m2 = rc[:, 64:80]
    w1 = rc[:, 80:96]
    w2 = rc[:, 96:112]
    e2 = rc[:, 112:128]
    ohg, tg_, eg = rs_
    ME, oh1, ME2, oh2, tA, tB = RT
    R = ["logits", "rc", "rs", "rt"]

    def rop(eng, fn, extra_r=()):
        P.op(eng, fn, reads=R + list(extra_r), writes=["rc", "rs", "rt"])

    def bc(ap2, n):
        return ap2.unsqueeze(2).to_broadcast([128, 16, n])

    rop("dve", lambda e: e.tensor_reduce(out=mg, in_=LG, axis=AX.X, op=ALU.max))
    rop("dve", lambda e: e.tensor_tensor(out=ohg, in0=LG, in1=bc(mg, 4), op=ALU.is_equal))
    rop("dve", lambda e: e.tensor_tensor(out=tg_, in0=LG, in1=bc(mg, 4), op=ALU.subtract))
    rop("act", lambda e: e.activation(out=eg, in_=tg_, func=AF.Exp))
    rop("dve", lambda e: e.tensor_reduce(out=sg_, in_=eg, axis=AX.X, op=ALU.add))
    rop("dve", lambda e: e.reciprocal(out=pg, in_=sg_))
    rop("dve", lambda e: e.tensor_scalar(out=tg_, in0=ohg, scalar1=1e30, scalar2=-1e30, op0=ALU.mult, op1=ALU.add))
    rop("dve", lambda e: e.tensor_tensor(out=ME.rearrange("p t (g k) -> p t g k", g=4), in0=LE.rearrange("p t (g k) -> p t g k", g=4),
                                         in1=tg_.unsqueeze(3).to_broadcast([128, 16, 4, 4]), op=ALU.add))
    rop("dve", lambda e: e.tensor_reduce(out=m1, in_=ME, axis=AX.X, op=ALU.max))
    rop("dve", lambda e: e.tensor_tensor(out=oh1, in0=ME, in1=bc(m1, 16), op=ALU.is_equal))
    rop("dve", lambda e: e.scalar_tensor_tensor(out=ME2, in0=oh1, scalar=-1e30, in1=ME, op0=ALU.mult, op1=ALU.add))
    rop("dve", lambda e: e.tensor_reduce(out=m2, in_=ME2, axis=AX.X, op=ALU.max))
    rop("dve", lambda e: e.tensor_tensor(out=oh2, in0=ME2, in1=bc(m2, 16), op=ALU.is_equal))
    rop("dve", lambda e: e.tensor_tensor(out=e2, in0=m2, in1=m1, op=ALU.subtract))
    rop("act", lambda e: e.activation(out=e2, in_=e2, func=AF.Exp))
    rop("dve", lambda e: e.tensor_scalar(out=w1, in0=e2, scalar1=1.0, scalar2=None, op0=ALU.add))
    rop("dve", lambda e: e.reciprocal(out=w1, in_=w1))
    rop("dve", lambda e: e.tensor_tensor(out=w2, in0=e2, in1=w1, op=ALU.mult))
    rop("dve", lambda e: e.tensor_tensor(out=w1, in0=w1, in1=pg, op=ALU.mult))
    rop("dve", lambda e: e.tensor_tensor(out=w2, in0=w2, in1=pg, op=ALU.mult))

    Mb_te = Mb.rearrange("p (e t) -> p t e", e=16)
    rop("dve", lambda e: e.tensor_tensor(out=Mb_te, in0=oh1, in1=oh2, op=ALU.add), ["Mb"])
    P.op("pe", lambda e: e.matmul(pS[0][:, 0:256], UTb, Mb, start=True, stop=True), reads=R + ["UTb", "Mb"], writes=["pS0"])
    P.op("pe", lambda e: e.matmul(pS[1][:, 0:256], onesb, Mb, start=True, stop=True), reads=R + ["onesb", "Mb"], writes=["pS1"])
    cntS = ME.rearrange("p t e -> p (t e)")
    incl = ME2.rearrange("p t e -> p (t e)")
    posb = tA.rearrange("p t e -> p (t e)")
    tmpb = tB
    rop("dve", lambda e: e.tensor_copy(out=cntS, in_=pS[1][:, 0:256]), ["pS1"])
    rop("dve", lambda e: e.tensor_tensor_scan(out=incl, data0=ones16, data1=cntS, initial=0.0, op0=ALU.mult, op1=ALU.add), ["ones16"])
    rop("dve", lambda e: e.tensor_tensor(out=posb, in0=incl, in1=cntS, op=ALU.subtract))
    rop("dve", lambda e: e.tensor_tensor(out=posb.rearrange("p (e t) -> p e t", e=16)[:, 1:16, :], in0=posb.rearrange("p (e t) -> p e t", e=16)[:, 1:16, :],
                                         in1=incl.rearrange("p (e t) -> p e t", e=16)[:, 0:15, 15:16].to_broadcast([128, 15, 16]), op=ALU.subtract))
    rop("dve", lambda e: e.tensor_tensor(out=posb, in0=posb, in1=pS[0][:, 0:256], op=ALU.add), ["pS0"])
    pos_te = posb.rearrange("p (e t) -> p t e", e=16)
    DUMC = cols[:, 44:45]
    for k, ohk in enumerate((oh1, oh2)):
        pk = offf[:, k * 16:(k + 1) * 16]
        ek = rc[:, 0:16]
        ov = rc[:, 16:32]
        rop("dve", lambda e, ohk=ohk: e.tensor_tensor(out=tmpb, in0=ohk, in1=pos_te, op=ALU.mult))
        rop("dve", lambda e, pk=pk: e.tensor_reduce(out=pk, in_=tmpb, axis=AX.X, op=ALU.add), ["offf"])
        rop("dve", lambda e, ohk=ohk: e.tensor_tensor(out=tmpb, in0=ohk, in1=ECc, op=ALU.mult), ["ECc"])
        rop("dve", lambda e, ek=ek: e.tensor_reduce(out=ek, in_=tmpb, axis=AX.X, op=ALU.add))
        rop("dve", lambda e, pk=pk, ov=ov: e.tensor_scalar(out=ov, in0=pk, scalar1=float(CAP), scalar2=None, op0=ALU.is_ge), ["offf"])
        rop("dve", lambda e, pk=pk, ek=ek: e.tensor_tensor(out=pk, in0=pk, in1=ek, op=ALU.add), ["offf"])
        rop("dve", lambda e, pk=pk, ek=ek: e.tensor_scalar(out=ek, in0=pk, scalar1=-1.0, scalar2=DUMC, op0=ALU.mult, op1=ALU.add), ["offf", "cols"])
        rop("dve", lambda e, ek=ek, ov=ov: e.tensor_tensor(out=ek, in0=ek, in1=ov, op=ALU.mult))
        rop("dve", lambda e, pk=pk, ek=ek: e.tensor_tensor(out=pk, in0=pk, in1=ek, op=ALU.add), ["offf"])
    P.op("dve", lambda e: e.tensor_copy(out=offi, in_=offf), reads=R + ["offf"], writes=["offi"])
    if dbg:
        dbg_store("dbg_logits", logits, "logits", [128, NT * 20], view=lambda d: d.rearrange("p (t f) -> p t f", t=NT))
        dbg_store("dbg_off", offf, ["offf", "rt"], [128, 32])
        dbg_store("dbg_w", rc[:, 80:112], ["rc", "rt", "offi"], [128, 32])
    if upto <= 3.5:
        P.fence()
        return finish(nc, P, final_evs, out_d, dbg_d)

    I32 = mybir.dt.int32
    xs_keys = []
    sc_evs = []
    for i in range(NT):
        for k in range(2):
            key = ("Xs", i, k)
            xs_keys.append(key)
            ev_ = P.dma("pool", lambda e, i=i, k=k: e.indirect_dma_start(
                out=Xs_d, out_offset=bass.IndirectOffsetOnAxis(ap=offi[:, k * 16 + i:k * 16 + i + 1], axis=0),
                in_=xtok[:, i, :], in_offset=None),
                reads=[("xtok", i), "offi"], writes=[key], extra=sc_evs[-4:-3])
            sc_evs.append(ev_)
    P.fence()
    yz = yst[0]
    ya_ = Arena(A0, A0 + 32768)
    NY = 6
    yst = [ya_.take("ystr%d" % i, [128, D], F32) for i in range(NY)]
    c3 = Arena(R2, A1LO)
    xe = [c3.take("xe0", [128, NB, D], BF16)] * 2
    xeT = [c3.take("xeT%d" % i, [128, 8, CAP], BF16) for i in range(2)]
    hidT = [c3.take("hidT0", [128, 4, CAP], BF16)] * 2
    sgt = [c3.take("sgt%d" % i, [128, CAP], BF16) for i in range(2)]

    GU = [(pS[0], "pS0", pS[1], "pS1"), (pS[2], "pS2", pS[3], "pS3")]
    gun = [0]
    dn = [0]
    Dbanks = [(W0, "W0"), (W1, "W1")]
    ys_keys = []
    nys = [0]
    nexp = NEXP if upto >= 5 else (max(1, int(round((upto - 4) * 100))) if upto > 4 else 1)
    tbanks = [(pT, "pT"), (pT2, "pT2")]
    tn = [0]

    def stage_T_load(e_):
        P.dma("sp", lambda e: e.dma_start(out=xe[0], in_=Xs_d[e_ * CAP:(e_ + 1) * CAP, :].rearrange("(b p) f -> p b f", p=128)),
              reads=xs_keys, writes=[("xe", 0)])

    def stage_T_block(e_, b):
        s = e_ % 2
        xek = ("xe", 0)
        tb_, tk_ = tbanks[tn[0] % 2]
        tn[0] += 1
        for kc in range(8):
            P.op("pe", lambda e, tb_=tb_, kc=kc: e.transpose(tb_[:, kc * 128:(kc + 1) * 128], xe[0][:, b, kc * 128:(kc + 1) * 128], identb),
                 reads=[xek, "identb"], writes=[tk_], inc=(kc == 7))
        evac_copy(xeT[s][:, :, b * 128:(b + 1) * 128], tb_.rearrange("p (c t) -> p c t", c=8), [tk_], [("xeT", s, b)])

    def stage_T(e_):
        stage_T_load(e_)
        for b in range(NB):
            stage_T_block(e_, b)

    def stage_GU(e_):
        s = e_ % 2
        xetk = [("xeT", s, b) for b in range(NB)]
        hid = hidT[s]
        for ffc in range(4):
            pg_, kg, pu_, ku = GU[gun[0] % 2]
            gun[0] += 1
            for kc in range(8):
                P.op("pe", lambda e, pg_=pg_, kc=kc, ffc=ffc, s=s: e.matmul(
                    pg_[:, 0:CAP], wg[s][:, kc, ffc * 128:(ffc + 1) * 128], xeT[s][:, kc, :], start=(kc == 0), stop=(kc == 7)),
                    reads=[("wg", s)] + xetk, writes=[kg], inc=(kc == 7))
            for kc in range(8):
                P.op("pe", lambda e, pu_=pu_, kc=kc, ffc=ffc, s=s: e.matmul(
                    pu_[:, 0:CAP], wu[s][:, kc, ffc * 128:(ffc + 1) * 128], xeT[s][:, kc, :], start=(kc == 0), stop=(kc == 7)),
                    reads=[("wu", s)] + xetk, writes=[ku], inc=(kc == 7))
            sg2 = sgt[ffc % 2]
            sk = "sgt%d" % (ffc % 2)
            P.op("act", lambda e, pg_=pg_, sg2=sg2: e.activation(out=sg2, in_=pg_[:, 0:CAP], func=AF.Silu), reads=[kg], writes=[sk])
            P.op("dve", lambda e, pu_=pu_, sg2=sg2, hid=hid, ffc=ffc: e.tensor_tensor(out=hid[:, ffc, :], in0=sg2, in1=pu_[:, 0:CAP], op=ALU.mult),
                 reads=[sk, ku], writes=[("hidT", 0, ffc)])

    def stage_D_block(e_, b):
        s = e_ % 2
        hid = hidT[s]
        yb = yst[nys[0] % NY]
        ybk = ("ystr", nys[0] % NY)
        nys[0] += 1
        for half in range(2):
            bank, bkey = Dbanks[dn[0] % 2]
            dn[0] += 1
            for ffc in range(4):
                P.op("pe", lambda e, bank=bank, hid=hid, ffc=ffc, half=half: e.matmul(
                    bank, hid[:, ffc, b * 128:(b + 1) * 128], wd[s][:, ffc, half * 512:(half + 1) * 512], start=(ffc == 0), stop=(ffc == 3)),
                    reads=[("hidT", 0, ffc), ("wd", s)], writes=[bkey], inc=(ffc == 3))
            evac_copy(yb[:, half * 512:(half + 1) * 512], bank, [bkey], [ybk])
        yk = ("Ys", e_, b)
        ys_keys.append(yk)
        P.dma("sp", lambda e, yb=yb: e.dma_start(out=Ys_d[e_ * CAP + b * 128:e_ * CAP + (b + 1) * 128, :], in_=yb),
              reads=[ybk], writes=[yk])

    stage_T(0)
    for e_ in range(nexp):
        stage_GU(e_)
        if e_ + 1 < nexp:
            stage_T_load(e_ + 1)
        for b in range(NB):
            stage_D_block(e_, b)
            if e_ + 1 < nexp:
                stage_T_block(e_ + 1, b)
        if e_ + 2 < nexp:
            load_expert(e_ + 2)

    P.fence()
    c4 = Arena(R2, A1LO)
    G = [c4.take("G%d" % i, [128, D], F32) for i in range(4)]
    ng = [0]
    def combine_acc(i):
        hk = ("hbuf", i)
        for k in range(2):
            gb = G[ng[0] % 4]
            gk = ("G", ng[0] % 4)
            ng[0] += 1
            P.dma("pool", lambda e, k=k, gb=gb: e.indirect_dma_start(
                out=gb, out_offset=None, in_=Ys_d,
                in_offset=bass.IndirectOffsetOnAxis(ap=offi[:, k * 16 + i:k * 16 + i + 1], axis=0)),
                reads=ys_keys + ["Ysdummy", "offi"], writes=[gk])
            wk_ = rc[:, 80 + k * 16 + i:80 + k * 16 + i + 1]
            P.op("dve", lambda e, gb=gb, wk_=wk_: e.scalar_tensor_tensor(out=hbuf[:, i, :], in0=gb, scalar=wk_, in1=hbuf[:, i, :],
                                                                        op0=ALU.mult, op1=ALU.add),
                 reads=[gk, "rc", hk], writes=[hk])
        rms_stats(hbuf[:, i, :], hk, junk3, "junk3", 9, i, D)

    def combine_fin(i):
        hk = ("hbuf", i)
        P.op("dve", lambda e: e.scalar_tensor_tensor(out=hbuf[:, i, :], in0=hbuf[:, i, :], scalar=st(11, i), in1=gfbc, op0=ALU.mult, op1=ALU.mult),
             reads=[hk, ("stat", 9, i), "gfbc"], writes=[hk])
        final_evs.append(P.dma("sp", lambda e: e.dma_start(out=out_d[i * 128:(i + 1) * 128, :], in_=hbuf[:, i, :]), reads=[hk]))

    for i in range(NT):
        combine_acc(i)
        if i >= 1:
            combine_fin(i - 1)
    combine_fin(NT - 1)
    return finish(nc, P, final_evs, out_d, dbg_d)


def finish(nc, P, final_evs, out_d, dbg_d):
    P.emit(final_waits=final_evs)
    return nc


def prep_shared(inp):
    f = np.float32
    rep = lambda v, n=128: np.ascontiguousarray(np.broadcast_to(np.asarray(v, f).reshape(1, -1), (n, np.asarray(v).size)))
    colsT = lambda v: np.ascontiguousarray(np.asarray(v, f).reshape(-1, 128).T)
    sh = {}
    sh["w_in"] = np.ascontiguousarray(inp["w_in"][0], f)
    sh["w_out"] = np.ascontiguousarray(inp["w_out"][0], f)
    sh["weg"] = np.ascontiguousarray(inp["w_e_gate"][0], f)
    sh["weu"] = np.ascontiguousarray(inp["w_e_up"][0], f)
    sh["wed"] = np.ascontiguousarray(inp["w_e_down"][0], f)
    sh["g1bc"] = rep(inp["norm1_g"][0])
    sh["g2bc"] = rep(inp["norm2_g"][0])
    sh["gfbc"] = rep(inp["final_g"])
    sh["gattbc"] = rep(inp["g_att_out"][0])
    cols = np.zeros((128, 64), f)
    cw = np.asarray(inp["conv_w"][0], f)
    for c in range(4):
        for j in range(4):
            cols[:, c * 4 + j] = cw[j, c * 128:(c + 1) * 128]
    cols[:, 16:20] = colsT(inp["conv_b"][0])
    cols[:, 20:24] = colsT(inp["b_rg_a"][0].reshape(-1))
    cols[:, 24:28] = colsT(inp["b_rg_x"][0].reshape(-1))
    cols[:, 28:32] = colsT(inp["lru_lambda"][0])
    cols[:, 32:36] = colsT(inp["g_rec_out"][0])
    rb = np.asarray(inp["rel_bias"][0], f)
    cols[:, 36:44] = np.broadcast_to(rb[:, 256][None, :], (128, 8))
    sh["cols"] = cols
    bd = np.zeros((128, 1024), f)
    wa = np.asarray(inp["w_rg_a"][0], f)
    wx = np.asarray(inp["w_rg_x"][0], f)
    for c in range(4):
        for hh in range(2):
            bd[hh * 64:(hh + 1) * 64, c * 128 + hh * 64: c * 128 + (hh + 1) * 64] = wa[2 * c + hh]
            bd[hh * 64:(hh + 1) * 64, 512 + c * 128 + hh * 64: 512 + c * 128 + (hh + 1) * 64] = wx[2 * c + hh]
    sh["bd"] = bd
    kk = np.arange(128)[:, None]
    qq = np.arange(128)[None, :]
    idx_prev = np.clip(128 + qq - kk, -128, 128) + 128
    idx_diag = np.clip(qq - kk, -128, 128) + 128
    masked = (kk >= 64) & (qq < 64)
    braw = np.zeros((128, 8, 256), f)
    for h in range(8):
        braw[:, h, 0:128] = rb[h][idx_prev]
        d = rb[h][idx_diag].copy()
        d[masked] = -30000.0
        braw[:, h, 128:256] = d
    sh["braw"] = braw.reshape(128, 2048)
    sh["wr"] = np.ascontiguousarray(np.concatenate([inp["w_group"][0], inp["w_router"][0]], axis=1), f)
    sh["brbc"] = rep(np.concatenate([inp["b_group"][0], inp["b_router"][0]]))
    sh["identf"] = np.eye(128, dtype=f)
    CAP = 512
    sh["ecc"] = np.ascontiguousarray(np.broadcast_to((np.arange(16, dtype=f) * CAP)[None, None, :], (128, 16, 16)).reshape(128, 256))
    sh["utri"] = np.triu(np.ones((128, 128), f), 1)
    cols[:, 44] = 16 * CAP + np.arange(128, dtype=f)
    return sh


_NC_CACHE = {}


def kernel(**inputs):
    inp = {k: np.asarray(v) for k, v in inputs.items()}
    sh = prep_shared(inp)
    if "nc" not in _NC_CACHE:
        _NC_CACHE["nc"] = build_nc()
    nc = _NC_CACHE["nc"]
    x = np.asarray(inp["x"], np.float32)
    in_maps = []
    for b in range(8):
        m = dict(sh)
        m["x"] = np.ascontiguousarray(x[b])
        in_maps.append(m)
    res = run_bass_kernel_spmd(nc, in_maps, core_ids=list(range(8)))
    out = np.stack([np.asarray(res.results[b]["out"], np.float32) for b in range(8)], axis=0)
    return out
```

```python
from contextlib import ExitStack
import numpy as np
import concourse.bass as bass
import concourse.mybir as mybir
from concourse.bass_utils import run_bass_kernel_spmd

F32 = mybir.dt.float32
BF16 = mybir.dt.bfloat16
AF = mybir.ActivationFunctionType
ALU = mybir.AluOpType
AX = mybir.AxisListType

ENGS = ("pe", "act", "dve", "pool", "sp")
T = 2048
D = 1024
NT = 16
EPS = 1e-6
NEXP = 16


class Ev:
    __slots__ = ("kind", "eng", "idx", "sem", "val")

    def __init__(self, kind, eng=None, idx=None, sem=None, val=None):
        self.kind, self.eng, self.idx, self.sem, self.val = kind, eng, idx, sem, val


class Prog:
    def __init__(self, nc, n_dma_sems=16, same_engine_sync=True):
        self.nc = nc
        self.ops = {e: [] for e in ENGS}
        self.same_engine_sync = same_engine_sync
        self.n_dma_sems = n_dma_sems
        self.dma_sem_tot = [0] * n_dma_sems
        self.dma_rr = 0
        self.dma_rr2 = [0, 0]
        self.last_w = {}
        self.readers = {}

    def _deps_for(self, reads, writes):
        deps = []
        for k in reads:
            if k in self.last_w:
                deps.append(self.last_w[k])
        for k in writes:
            if k in self.last_w:
                deps.append(self.last_w[k])
            deps.extend(self.readers.get(k, ()))
        return deps

    def _commit(self, ev, reads, writes):
        for k in reads:
            self.readers.setdefault(k, []).append(ev)
        for k in writes:
            self.last_w[k] = ev
            self.readers[k] = []

    def op(self, eng, fn, reads=(), writes=(), inc=True, extra=()):
        deps = self._deps_for(reads, writes) + list(extra)
        idx = len(self.ops[eng])
        self.ops[eng].append(dict(fn=fn, deps=deps, inc=inc, dma=None))
        ev = Ev("eng", eng=eng, idx=idx)
        self._commit(ev, reads, writes)
        return ev

    def dma(self, eng, fn, reads=(), writes=(), extra=()):
        deps = self._deps_for(reads, writes) + list(extra)
        half = self.n_dma_sems // 2
        r = 1 if eng == "pool" else 0
        s = r * half + self.dma_rr2[r]
        self.dma_rr2[r] = (self.dma_rr2[r] + 1) % half
        prev = self.dma_sem_tot[s]
        if prev:
            deps.append(Ev("dma", sem=s, val=prev))
        self.dma_sem_tot[s] = prev + 16
        ev = Ev("dma", sem=s, val=prev + 16)
        self.ops[eng].append(dict(fn=fn, deps=deps, inc=False, dma=s))
        self._commit(ev, reads, writes)
        return ev

    def fence(self):
        evs = []
        for e in ENGS:
            ops = self.ops[e]
            j = len(ops) - 1
            while j >= 0 and (ops[j].get("nop") or ops[j]["dma"] is not None):
                j -= 1
            if j >= 0:
                assert ops[j]["inc"], "fence: last op on %s has no inc" % e
                evs.append(Ev("eng", eng=e, idx=j))
        for s in range(self.n_dma_sems):
            if self.dma_sem_tot[s]:
                evs.append(Ev("dma", sem=s, val=self.dma_sem_tot[s]))
        for e in ENGS:
            self.ops[e].append(dict(fn=None, deps=list(evs), inc=False, dma=None, nop=True))

    def emit(self, final_waits=()):
        nc = self.nc
        with ExitStack() as st:
            esem = {e: st.enter_context(nc.semaphore("c_" + e)) for e in ENGS}
            dsem = [st.enter_context(nc.semaphore("d%d" % i)) for i in range(self.n_dma_sems)]
            cum = {}
            for e in ENGS:
                c = 0
                arr = []
                for o in self.ops[e]:
                    if o["inc"]:
                        c += 1
                    arr.append(c)
                cum[e] = arr
            ninc = {e: (cum[e][-1] if cum[e] else 0) for e in ENGS}

            def resolve(ev):
                if ev.kind == "dma":
                    return dsem[ev.sem], ev.val
                o = self.ops[ev.eng][ev.idx]
                v = cum[ev.eng][ev.idx]
                if not o["inc"]:
                    v += 1
                    assert v <= ninc[ev.eng], "event on non-inc op without later inc"
                return esem[ev.eng], v

            block = st.enter_context(nc.Block())

            def make(ename):
                ops = self.ops[ename]

                def body(eng):
                    waited = {}
                    for o in ops:
                        need = {}
                        for ev in o["deps"]:
                            if ev.kind == "eng" and ev.eng == ename and not o.get("nop") and (
                                    ename == "pe" or not self.same_engine_sync):
                                continue
                            sem, v = resolve(ev)
                            key = id(sem)
                            if v > need.get(key, (None, 0))[1]:
                                need[key] = (sem, v)
                        for key, (sem, v) in need.items():
                            if waited.get(key, 0) >= v:
                                continue
                            eng.wait_ge(sem, v)
                            waited[key] = v
                        if o.get("nop"):
                            continue
                        ins = o["fn"](eng)
                        if o["dma"] is not None:
                            ins.then_inc(dsem[o["dma"]], 16)
                        elif o["inc"]:
                            ins.then_inc(esem[ename], 1)
                    if ename == "sp":
                        for ev in final_waits:
                            sem, v = resolve(ev)
                            eng.wait_ge(sem, v)
                return body

            block.tensor(make("pe"))
            block.scalar(make("act"))
            block.vector(make("dve"))
            block.gpsimd(make("pool"))
            block.sync(make("sp"))


SB_LO = 16512
SB_HI = 229344
PERSIST = 8192
A0 = SB_LO + PERSIST


def build_nc(dbg=False, upto=99):
    nc = bass.Bass("TRN2", target_bir_lowering=False)

    def din(name, shape, dt=F32):
        return nc.dram_tensor(name, list(shape), dt, kind="ExternalInput").ap()

    x_d = din("x", [T, D])
    win_d = din("w_in", [D, 2560])
    wout_d = din("w_out", [D, D])
    weg_d = din("weg", [NEXP, D, 512])
    weu_d = din("weu", [NEXP, D, 512])
    wed_d = din("wed", [NEXP, 512, D])
    g1_d = din("g1bc", [128, D])
    g2_d = din("g2bc", [128, D])
    gf_d = din("gfbc", [128, D])
    gatt_d = din("gattbc", [128, 512])
    cols_d = din("cols", [128, 64])
    bd_d = din("bd", [128, 1024])
    braw_d = din("braw", [128, 2048])
    wr_d = din("wr", [D, 20])
    brbc_d = din("brbc", [128, 20])
    id_d = din("identf", [128, 128])
    ec_d = din("ecc", [128, 256])
    ut_d = din("utri", [128, 128])
    out_d = nc.dram_tensor("out", [T, D], F32, kind="ExternalOutput").ap()
    dbg_d = {}

    def dout(name, shape):
        dbg_d[name] = nc.dram_tensor(name, list(shape), F32, kind="ExternalOutput").ap()
        return dbg_d[name]

    def sb_at(name, shape, dt, off):
        nbytes = int(np.prod(shape[1:])) * (2 if dt == BF16 else 4)
        assert off % 32 == 0, (name, off)
        assert SB_LO <= off and off + nbytes <= SB_HI, (name, off, nbytes)
        return nc.alloc_sbuf_tensor_at(name, list(shape), dt, offset=off).ap()

    class Arena:
        def __init__(self, lo, hi):
            self.lo, self.hi, self.cur = lo, hi, lo

        def take(self, name, shape, dt):
            nbytes = int(np.prod(shape[1:])) * (2 if dt == BF16 else 4)
            nbytes = (nbytes + 31) // 32 * 32
            off = self.cur
            assert off + nbytes <= self.hi, ("arena overflow", name, off, nbytes, self.hi)
            self.cur += nbytes
            return sb_at(name, shape, dt, off)

    pers = Arena(SB_LO, A0)
    identf = pers.take("identf", [128, 128], F32)
    onesf = pers.take("onesf", [128, 128], F32)
    identb = pers.take("identb", [128, 128], BF16)
    cols = pers.take("colsb", [128, 64], F32)
    stat = pers.take("stat", [128, 16 * 12], F32)
    brbc = pers.take("brbc", [128, 20], F32)
    wr = pers.take("wr", [128, 8, 20], F32)
    logits = pers.take("logits", [128, NT, 20], F32)
    uhalo = pers.take("uhalo", [128, 4, 4], F32)
    hprev = pers.take("hprev", [128, 4], F32)
    scal = pers.take("scal", [128, 32], F32)

    CW = lambda c, j: cols[:, c * 4 + j: c * 4 + j + 1]
    CB = lambda c: cols[:, 16 + c: 17 + c]
    BA = lambda c: cols[:, 20 + c: 21 + c]
    BX = lambda c: cols[:, 24 + c: 25 + c]
    LAM = cols[:, 28:32]
    GREC = lambda c: cols[:, 32 + c: 33 + c]
    BFAR = lambda h: cols[:, 36 + h: 37 + h]
    CL = lambda c: scal[:, 8 + c: 9 + c]
    CL2 = lambda c: scal[:, 12 + c: 13 + c]

    def st(k, i):
        return stat[:, k * 16 + i: k * 16 + i + 1]

    xnT = sb_at("xnT", [128, 8, T], BF16, A0 + 0)
    mixT = sb_at("mixT", [128, 8, T], BF16, A0 + 32768)
    qT = sb_at("qT", [128, 4, T], BF16, A0 + 65536)
    kT = sb_at("kT", [128, 4, T], BF16, A0 + 81920)
    vaug = sb_at("vaug", [128, NT, 8, 65], BF16, A0 + 98304)
    A1LO = A0 + 114944

    pT = nc.alloc_psum_tensor("pT", [128, 1024], BF16).ap()
    pW = nc.alloc_psum_tensor("pW", [128, 1024], F32).ap()
    pS = [nc.alloc_psum_tensor("pS%d" % i, [128, 512], F32).ap() for i in range(4)]
    pT2 = nc.alloc_psum_tensor("pT2", [128, 1024], BF16).ap()
    W0, W1 = pW[:, 0:512], pW[:, 512:1024]

    P = Prog(nc)
    final_evs = []

    def dbg_store(name, ap_sb, key, shape, view=None):
        if not dbg:
            return
        d = dout(name, shape)
        keys = key if isinstance(key, list) else [key]
        final_evs.append(P.dma("pool", lambda e, d=d, a=ap_sb: e.dma_start(out=(d if view is None else view(d)), in_=a),
                               reads=keys))

    P.dma("sp", lambda e: e.dma_start(out=identf, in_=id_d), writes=["identf"])
    P.dma("sp", lambda e: e.dma_start(out=cols, in_=cols_d), writes=["cols"])
    P.dma("sp", lambda e: e.dma_start(out=brbc, in_=brbc_d), writes=["brbc"])
    P.dma("sp", lambda e: e.dma_start(out=wr, in_=wr_d.rearrange("(kc p) n -> p kc n", p=128)), writes=["wr"])
    P.dma("pool", lambda e: e.dma_start(out=identb, in_=id_d), writes=["identb"])
    P.op("pool", lambda e: e.memset(onesf, 1.0), writes=["onesf"])
    P.op("pool", lambda e: e.memset(stat, 0.0), writes=["stat0"])
    P.op("pool", lambda e: e.memset(uhalo, 0.0), writes=["uhalo"])
    P.op("pool", lambda e: e.memset(hprev, 0.0), writes=["hprev"])
    P.op("pool", lambda e: e.memset(vaug[:, :, :, 64:65], 1.0), writes=["vaug1"])
    P.op("act", lambda e: e.activation(out=scal[:, 0:4], in_=LAM, func=AF.Exp, scale=-1.0), reads=["cols"], writes=["scal"])
    P.op("act", lambda e: e.activation(out=scal[:, 4:8], in_=scal[:, 0:4], func=AF.Ln, bias=1.0), reads=["scal"], writes=["scal"])
    P.op("dve", lambda e: e.tensor_scalar(out=scal[:, 8:12], in0=scal[:, 4:8], scalar1=-8.0, scalar2=None, op0=ALU.mult), reads=["scal"], writes=["scal"])
    P.op("dve", lambda e: e.tensor_scalar(out=scal[:, 12:16], in0=scal[:, 4:8], scalar1=-16.0, scalar2=None, op0=ALU.mult), reads=["scal"], writes=["scal"])

    def rms_stats(src_ap, src_key, junk_ap, junk_key, k0, i, n):
        sk = ("stat", k0, i)
        P.op("act", lambda e: e.activation(out=junk_ap, in_=src_ap, func=AF.Square, accum_out=st(k0, i)),
             reads=[src_key, "stat0"], writes=[junk_key, sk])
        P.op("act", lambda e: e.activation(out=st(k0 + 1, i), in_=st(k0, i), func=AF.Ln, scale=1.0 / n, bias=EPS),
             reads=[sk], writes=[sk])
        P.op("act", lambda e: e.activation(out=st(k0 + 2, i), in_=st(k0 + 1, i), func=AF.Exp, scale=-0.5),
             reads=[sk], writes=[sk])

    a1 = Arena(A1LO, SB_HI)
    wring = [a1.take("wring%d" % i, [128, 8, 128], BF16) for i in range(2)]
    wug = a1.take("wug", [128, 8, 8, 128], BF16)
    junk = a1.take("junk", [128, D], BF16)
    bd = a1.take("bd", [128, 1024], F32)
    Ub = a1.take("Ub", [128, 4, 516], F32)
    Gt = a1.take("Gt", [128, 4, 512], F32)
    UC = a1.take("UC", [128, 4, 512], F32)
    RR = a1.take("RR", [128, 4, 512], F32)
    IG = a1.take("IG", [128, 4, 512], F32)
    A2 = a1.take("A2", [128, 4, 512], F32)
    GT = a1.take("GT", [128, 4, 512], F32)
    rb = a1.take("rb", [128, 512], F32)
    n1 = Arena(A0 + 32768 + 16384, A0 + 65536)
    xstage = [n1.take("xstage%d" % i, [128, D], F32) for i in range(2)]
    xs = [n1.take("xs%d" % i, [128, D], BF16) for i in range(2)]
    g1bc = n1.take("g1bc", [128, D], F32)

    P.dma("sp", lambda e: e.dma_start(out=g1bc, in_=g1_d), writes=["g1bc"])
    P.dma("sp", lambda e: e.dma_start(out=bd, in_=bd_d), writes=["bd"])

    win_v = win_d.rearrange("(kc p) n -> p kc n", p=128)
    xnT_all = [("xnT", i) for i in range(NT)]

    def norm1_tile(i):
        xst = xstage[i % 2]
        kx = "xstage%d" % (i % 2)
        kxs = "xs%d" % (i % 2)
        P.dma("sp", lambda e: e.dma_start(out=xst, in_=x_d[i * 128:(i + 1) * 128, :]), writes=[kx])
        rms_stats(xst, kx, junk, "junk", 0, i, D)
        P.op("dve", lambda e: e.scalar_tensor_tensor(out=xs[i % 2], in0=xst, scalar=st(2, i), in1=g1bc, op0=ALU.mult, op1=ALU.mult),
             reads=[kx, ("stat", 0, i), "g1bc"], writes=[kxs])
        tb_, tk_ = (pT, "pT") if i % 2 == 0 else (pT2, "pT2")
        for c in range(8):
            P.op("pe", lambda e, c=c: e.transpose(tb_[:, c * 128:(c + 1) * 128], xs[i % 2][:, c * 128:(c + 1) * 128], identb),
                 reads=[kxs, "identb"], writes=[tk_], inc=(c == 7))
        P.op("act", lambda e: e.copy(out=xnT[:, :, i * 128:(i + 1) * 128], in_=tb_.rearrange("p (c t) -> p c t", c=8)),
             reads=[tk_], writes=[("xnT", i)])

    mm_banks = [W0, W1]
    mm_keys = ["W0", "W1"]
    mmc = [0]
    evc = [0]

    def next_bank():
        b = mmc[0] % 2
        mmc[0] += 1
        return mm_banks[b], mm_keys[b]

    def evac_copy(out_ap, in_ap, reads, writes, eng=None):
        if eng is None:
            eng = "act" if evc[0] % 2 == 0 else "dve"
            evc[0] += 1
        if eng == "act":
            P.op("act", lambda e: e.copy(out=out_ap, in_=in_ap), reads=reads, writes=writes)
        else:
            P.op("dve", lambda e: e.tensor_copy(out=out_ap, in_=in_ap), reads=reads, writes=writes)

    nring = [0]

    def qkv_chunk(j, tb):
        slot = j % 2
        wk = "wring%d" % slot
        if j < 8:
            dstT, dkey = (qT, "qT") if j < 4 else (kT, "kT")
            bank, bkey = next_bank()
            for kc in range(8):
                P.op("pe", lambda e, kc=kc: e.matmul(bank, wring[slot][:, kc, :], xnT[:, kc, tb * 512:(tb + 1) * 512], start=(kc == 0), stop=(kc == 7)),
                     reads=[wk] + xnT_all[tb * 4:(tb + 1) * 4], writes=[bkey], inc=(kc == 7))
            evac_copy(dstT[:, j % 4, tb * 512:(tb + 1) * 512], bank, [bkey], [(dkey, j % 4, tb)], eng="act")
        else:
            jj = j - 8
            bank, bkey = next_bank()
            for tt in range(4):
                i = tb * 4 + tt
                for kc in range(8):
                    P.op("pe", lambda e, kc=kc, i=i, tt=tt: e.matmul(bank[:, tt * 128:(tt + 1) * 128], xnT[:, kc, i * 128:(i + 1) * 128], wring[slot][:, kc, :],
                                                                    start=(kc == 0), stop=(kc == 7)),
                         reads=[wk, ("xnT", i)], writes=[bkey], inc=(kc == 7))
            evac_copy(vaug[:, tb * 4:(tb + 1) * 4, 2 * jj:2 * jj + 2, 0:64], bank.rearrange("p (t h d) -> p t h d", t=4, h=2),
                      [bkey], [("vaug", jj, tb)], eng="act")

    def load_wchunk(j):
        slot = j % 2
        col0 = 1024 + j * 128
        P.dma("pool", lambda e: e.dma_start(out=wring[slot], in_=win_v[:, :, col0:col0 + 128]), writes=["wring%d" % slot])

    for cc in range(8):
        P.dma("pool", lambda e, cc=cc: e.dma_start(out=wug[:, cc, :, :], in_=win_v[:, :, cc * 128:(cc + 1) * 128]), writes=[("wug", cc)])

    Gbanks = [(pS[0], "pS0", pS[1], "pS1"), (pS[2], "pS2", pS[3], "pS3")]
    gbn = [0]
    FL = lambda t: t.rearrange("p c t -> p (c t)")

    for i in range(4):
        norm1_tile(i)
    for tb in range(4):
        tsl = slice(tb * 512, (tb + 1) * 512)
        for c in range(4):
            for which in range(2):
                bank, bkey = next_bank()
                cc = which * 4 + c
                for kc in range(8):
                    P.op("pe", lambda e, bank=bank, cc=cc, kc=kc, tsl=tsl: e.matmul(bank, wug[:, cc, kc, :], xnT[:, kc, tsl], start=(kc == 0), stop=(kc == 7)),
                         reads=[("wug", cc)] + xnT_all[tb * 4:(tb + 1) * 4], writes=[bkey], inc=(kc == 7))
                if which == 0:
                    P.op("pool", lambda e, c=c: e.tensor_copy(out=Ub[:, c, 1:4], in_=uhalo[:, c, 1:4]), reads=["uhalo"], writes=[("Ub", c)])
                    P.op("act", lambda e, bank=bank, c=c: e.copy(out=Ub[:, c, 4:516], in_=bank), reads=[bkey, ("Ub", c)], writes=[("Ub", c)])
                    P.op("pool", lambda e, c=c: e.tensor_copy(out=uhalo[:, c, 1:4], in_=Ub[:, c, 513:516]), reads=[("Ub", c)], writes=["uhalo"])
                else:
                    P.op("dve", lambda e, bank=bank, c=c: e.tensor_copy(out=Gt[:, c, :], in_=bank), reads=[bkey], writes=[("Gt", c)])
        if tb + 1 < 4:
            for i in range(4 * (tb + 1), 4 * (tb + 2)):
                norm1_tile(i)
        for c in range(4):
            P.op("pool", lambda e, c=c: e.tensor_tensor(out=GT[:, c, :], in0=Gt[:, c, :], in1=Gt[:, c, :], op=ALU.mult), reads=[("Gt", c)], writes=[("GT", c)])
            P.op("pool", lambda e, c=c: e.tensor_scalar(out=GT[:, c, :], in0=GT[:, c, :], scalar1=0.044715, scalar2=1.0, op0=ALU.mult, op1=ALU.add),
                 reads=[("GT", c)], writes=[("GT", c)])
            P.op("pool", lambda e, c=c: e.tensor_tensor(out=GT[:, c, :], in0=GT[:, c, :], in1=Gt[:, c, :], op=ALU.mult), reads=[("GT", c), ("Gt", c)], writes=[("GT", c)])
        for c in range(4):
            P.op("dve", lambda e, c=c: e.tensor_scalar(out=UC[:, c, :], in0=Ub[:, c, 4:516], scalar1=CW(c, 3), scalar2=CB(c), op0=ALU.mult, op1=ALU.add),
                 reads=[("Ub", c), "cols"], writes=[("UC", c)])
            for k in (1, 2, 3):
                P.op("dve", lambda e, c=c, k=k: e.scalar_tensor_tensor(out=UC[:, c, :], in0=Ub[:, c, 4 - k:516 - k], scalar=CW(c, 3 - k), in1=UC[:, c, :],
                                                                       op0=ALU.mult, op1=ALU.add), reads=[("Ub", c), "cols", ("UC", c)], writes=[("UC", c)])
        for j in range(0, 4):
            load_wchunk(j)
            qkv_chunk(j, tb)
        for c in range(4):
            pr, kr, pi_, ki = Gbanks[gbn[0] % 2]
            gbn[0] += 1
            P.op("pe", lambda e, c=c, pr=pr: e.matmul(pr, bd[:, c * 128:(c + 1) * 128], UC[:, c, :], start=True, stop=True), reads=["bd", ("UC", c)], writes=[kr])
            P.op("pe", lambda e, c=c, pi_=pi_: e.matmul(pi_, bd[:, 512 + c * 128:512 + (c + 1) * 128], UC[:, c, :], start=True, stop=True),
                 reads=["bd", ("UC", c)], writes=[ki])
            P.op("act", lambda e, c=c, pr=pr: e.activation(out=RR[:, c, :], in_=pr, func=AF.Sigmoid, bias=BA(c)), reads=[kr, "cols"], writes=[("RR", c)])
            P.op("act", lambda e, c=c, pi_=pi_: e.activation(out=IG[:, c, :], in_=pi_, func=AF.Sigmoid, bias=BX(c)), reads=[ki, "cols"], writes=[("IG", c)])
        for c in range(4):
            P.op("act", lambda e, c=c: e.activation(out=GT[:, c, :], in_=GT[:, c, :], func=AF.Sigmoid, scale=1.5957691216057308), reads=[("GT", c)], writes=[("GT", c)])
        for c in range(4):
            P.op("act", lambda e, c=c: e.activation(out=A2[:, c, :], in_=RR[:, c, :], func=AF.Exp, scale=CL2(c)), reads=[("RR", c), "scal"], writes=[("A2", c)])
            P.op("act", lambda e, c=c: e.activation(out=RR[:, c, :], in_=RR[:, c, :], func=AF.Exp, scale=CL(c)), reads=[("RR", c), "scal"], writes=[("RR", c)])
        for j in range(4, 7):
            load_wchunk(j)
            qkv_chunk(j, tb)
        allk = lambda n: [(n, c) for c in range(4)]
        P.op("dve", lambda e: e.tensor_scalar(out=FL(A2), in0=FL(A2), scalar1=-1.0, scalar2=1.0, op0=ALU.mult, op1=ALU.add), reads=allk("A2"), writes=allk("A2"))
        P.op("act", lambda e: e.activation(out=FL(A2), in_=FL(A2), func=AF.Ln), reads=allk("A2"), writes=allk("A2"))
        P.op("act", lambda e: e.activation(out=FL(A2), in_=FL(A2), func=AF.Exp, scale=0.5), reads=allk("A2"), writes=allk("A2"))
        for j in range(7, 11):
            load_wchunk(j)
            qkv_chunk(j, tb)
        P.op("dve", lambda e: e.tensor_tensor(out=FL(IG), in0=FL(IG), in1=FL(UC), op=ALU.mult), reads=allk("IG") + allk("UC"), writes=allk("IG"))
        P.op("dve", lambda e: e.tensor_tensor(out=FL(IG), in0=FL(IG), in1=FL(A2), op=ALU.mult), reads=allk("IG") + allk("A2"), writes=allk("IG"))
        for c in range(4):
            P.op("dve", lambda e, c=c: e.tensor_tensor_scan(out=A2[:, c, :], data0=RR[:, c, :], data1=IG[:, c, :], initial=hprev[:, c:c + 1],
                                                            op0=ALU.mult, op1=ALU.add), reads=[("RR", c), ("IG", c), "hprev", ("A2", c)], writes=[("A2", c)])
            P.op("pool", lambda e, c=c: e.tensor_copy(out=hprev[:, c:c + 1], in_=A2[:, c, 511:512]), reads=[("A2", c)], writes=["hprev"])
        P.op("dve", lambda e: e.tensor_tensor(out=FL(GT), in0=FL(GT), in1=FL(Gt), op=ALU.mult), reads=allk("GT") + allk("Gt"), writes=allk("GT"))
        P.op("dve", lambda e: e.tensor_tensor(out=FL(GT), in0=FL(GT), in1=FL(A2), op=ALU.mult), reads=allk("GT") + allk("A2"), writes=allk("GT"))
        P.op("act", lambda e: e.activation(out=FL(UC), in_=FL(GT), func=AF.Square), reads=allk("GT") + allk("UC"), writes=allk("UC"))
        pr, kr, _, _ = Gbanks[gbn[0] % 2]
        gbn[0] += 1
        for c in range(4):
            P.op("pe", lambda e, c=c, pr=pr: e.matmul(pr, onesf, UC[:, c, :], start=(c == 0), stop=(c == 3)), reads=["onesf", ("UC", c)], writes=[kr], inc=(c == 3))
        P.op("act", lambda e, pr=pr: e.activation(out=rb, in_=pr, func=AF.Ln, scale=1.0 / 512, bias=EPS), reads=[kr], writes=["rb"])
        P.op("act", lambda e: e.activation(out=rb, in_=rb, func=AF.Exp, scale=-0.5), reads=["rb"], writes=["rb"])
        for j in range(11, 12):
            load_wchunk(j)
            qkv_chunk(j, tb)
        for c in range(4):
            P.op("dve", lambda e, c=c, tsl=tsl: e.scalar_tensor_tensor(out=mixT[:, c, tsl], in0=GT[:, c, :], scalar=GREC(c), in1=rb, op0=ALU.mult, op1=ALU.mult),
                 reads=[("GT", c), "rb", "cols"], writes=[("mixT", c, tb)])
    if dbg:
        dbg_store("dbg_xnT", xnT, xnT_all, [128, 8 * T], view=lambda d: d.rearrange("p (c t) -> p c t", c=8))
        dbg_store("dbg_mixrec", mixT[:, 0:4, :], [("mixT", c, tb) for c in range(4) for tb in range(4)], [128, 4 * T], view=lambda d: d.rearrange("p (c t) -> p c t", c=4))
        dbg_store("dbg_qT", qT, [("qT", c, tb) for c in range(4) for tb in range(4)], [128, 4 * T], view=lambda d: d.rearrange("p (c t) -> p c t", c=4))
        dbg_store("dbg_kT", kT, [("kT", c, tb) for c in range(4) for tb in range(4)], [128, 4 * T], view=lambda d: d.rearrange("p (c t) -> p c t", c=4))
        dbg_store("dbg_v", vaug, [("vaug", c, tb) for c in range(4) for tb in range(4)] + ["vaug1"], [128, NT * 8 * 65], view=lambda d: d.rearrange("p (t h d) -> p t h d", t=NT, h=8))

    P.fence()
    if upto <= 1:
        return finish(nc, P, final_evs, out_d, dbg_d)

    CAP = 512
    NB = CAP // 128
    RROWS = NEXP * CAP + 128
    Xs_d = nc.dram_tensor("Xs_scr", [RROWS, D], BF16).ap()
    Ys_d = nc.dram_tensor("Ys_scr", [RROWS, D], F32).ap()
    a2r = Arena(A0, A0 + 32768)
    Eb = [a2r.take("E%d" % i, [128, 640], BF16) for i in range(2)]
    yatt = [a2r.take("yatt%d" % i, [128, 512], F32) for i in range(2)]
    yan = [a2r.take("yan%d" % i, [128, 512], BF16) for i in range(2)]
    braw = a2r.take("braw", [128, 8, 256], F32)
    bias8 = a2r.take("bias8", [128, 8, 256], BF16)
    gattbc = a2r.take("gattbc", [128, 512], F32)
    junk2 = a2r.take("junk2", [128, 512], BF16)
    rden = a2r.take("rden", [128, 16], F32)
    hbuf = sb_at("hbuf", [128, NT, D], F32, A1LO)
    wout = sb_at("wout", [128, 8, D], BF16, A1LO + 65536)

    zt = a2r.take("zt", [128, D], BF16)
    P.op("pool", lambda e: e.memset(zt, 0.0), writes=["zt"])
    P.dma("sp", lambda e: e.dma_start(out=braw, in_=braw_d.rearrange("p (h k) -> p h k", h=8)), writes=["braw"])
    P.dma("sp", lambda e: e.dma_start(out=gattbc, in_=gatt_d), writes=["gattbc"])
    P.op("dve", lambda e: e.tensor_scalar(out=bias8, in0=braw, scalar1=8.0, scalar2=None, op0=ALU.mult), reads=["braw"], writes=["bias8"])
    P.dma("pool", lambda e: e.dma_start(out=wout, in_=wout_d.rearrange("(kc p) n -> p kc n", p=128)), writes=["wout"])
    for i in range(NT):
        P.dma("sp", lambda e, i=i: e.dma_start(out=hbuf[:, i, :], in_=x_d[i * 128:(i + 1) * 128, :]), writes=[("hbuf", i)])
    for r in range(RROWS // 128):
        P.dma("sp", lambda e, r=r: e.dma_start(out=Xs_d[r * 128:(r + 1) * 128, :], in_=zt), reads=["zt"], writes=[("Xs0", r)])

    units = [(i, h) for i in range(NT) for h in range(8)]
    NU = len(units)
    Sbanks = [(pS[0], "pS0", pS[1], "pS1"), (pS[2], "pS2", pS[3], "pS3")]
    Obanks = [(W0, "W0"), (W1, "W1")]

    def tiles_of(i):
        return list(range(max(0, i - 4), i + 1))

    def S1(n):
        i, h = units[n]
        psA, kA, psB, kB = Sbanks[n % 2]
        pb = (h % 2) * 64
        qh = qT[pb:pb + 64, h // 2, i * 128:(i + 1) * 128]
        qkeys = [("qT", h // 2, i // 4)]
        lo = 0 if i >= 1 else 128
        P.op("pe", lambda e: e.matmul(psB[:, lo:256], identb, bias8[:, h, lo:256], start=True, stop=False),
             reads=["identb", "bias8"], writes=[kB], inc=False)
        near = [t for t in (i - 1, i) if t >= 0]
        for idx, t in enumerate(near):
            s = t - (i - 4)
            kh = kT[pb:pb + 64, h // 2, t * 128:(t + 1) * 128]
            last = (idx == len(near) - 1)
            P.op("pe", lambda e, s=s, kh=kh, last=last: e.matmul(psB[:, (s - 3) * 128:(s - 2) * 128], kh, qh, start=False, stop=last),
                 reads=qkeys + [("kT", h // 2, t // 4)], writes=[kB], inc=last)
        far = [t for t in (i - 4, i - 3, i - 2) if t >= 0]
        for idx, t in enumerate(far):
            s = t - (i - 4)
            kh = kT[pb:pb + 64, h // 2, t * 128:(t + 1) * 128]
            last = (idx == len(far) - 1)
            P.op("pe", lambda e, s=s, kh=kh: e.matmul(psA[:, s * 128:(s + 1) * 128], kh, qh, start=True, stop=True),
                 reads=qkeys + [("kT", h // 2, t // 4)], writes=[kA], inc=last)

    def S2(n):
        i, h = units[n]
        psA, kA, psB, kB = Sbanks[n % 2]
        E = Eb[n % 2]
        ek = "E%d" % (n % 2)
        far = [t for t in (i - 4, i - 3, i - 2) if t >= 0]
        if far:
            s0 = far[0] - (i - 4)
            P.op("act", lambda e: e.activation(out=E[:, s0 * 128:384], in_=psA[:, s0 * 128:384], func=AF.Exp, bias=BFAR(h), scale=0.125),
                 reads=[kA, "cols"], writes=[ek + "a"])
        lo = 0 if i >= 1 else 128
        P.op("act", lambda e: e.activation(out=E[:, 384 + lo:640], in_=psB[:, lo:256], func=AF.Exp, scale=0.125),
             reads=[kB], writes=[ek + "b"])

    def S3(n):
        i, h = units[n]
        E = Eb[n % 2]
        ek = "E%d" % (n % 2)
        po, pk = Obanks[n % 2]
        ts = tiles_of(i)
        first = True
        for t in ts:
            s = t - (i - 4)
            last = (t == i)
            ekey = ek + ("a" if s < 3 else "b")
            vkeys = [("vaug", h // 2, t // 4), "vaug1"]
            if s == 0:
                P.op("pe", lambda e, t=t: e.matmul(po[:, 0:65], E[64:128, 0:128], vaug[64:128, t, h, :], start=True, stop=False),
                     reads=[ekey] + vkeys, writes=[pk], inc=False)
                P.op("pe", lambda e, t=t: e.matmul(po[0:64, 0:65], E[0:64, 0:64], vaug[0:64, t, h, :], start=False, stop=False),
                     reads=[ekey] + vkeys, writes=[pk], inc=False)
                first = False
            else:
                P.op("pe", lambda e, t=t, s=s, first=first, last=last: e.matmul(po[:, 0:65], E[:, s * 128:(s + 1) * 128], vaug[:, t, h, :],
                                                                                 start=first, stop=last),
                     reads=[ekey] + vkeys, writes=[pk], inc=last)
                first = False

    def S4(n):
        i, h = units[n]
        po, pk = Obanks[n % 2]
        ya = yatt[i % 2]
        P.op("dve", lambda e: e.reciprocal(out=rden[:, h:h + 1], in_=po[:, 64:65]), reads=[pk], writes=["rden"])
        P.op("dve", lambda e: e.tensor_scalar(out=ya[:, h * 64:(h + 1) * 64], in0=po[:, 0:64], scalar1=rden[:, h:h + 1], scalar2=None, op0=ALU.mult),
             reads=[pk, "rden"], writes=[("yatt", i % 2)])
        if h == 7:
            yk = ("yatt", i % 2)

            def blk_norm(i=i, ya=ya, yk=yk):
                rms_stats(ya, yk, junk2, "junk2", 3, i, 512)
                P.op("dve", lambda e: e.scalar_tensor_tensor(out=yan[i % 2], in0=ya, scalar=st(5, i), in1=gattbc, op0=ALU.mult, op1=ALU.mult),
                     reads=[yk, ("stat", 3, i), "gattbc"], writes=[("yan", i % 2)])
                deferred.append([2, blk_end])

            def blk_end(i=i):
                for c in range(4):
                    P.op("pe", lambda e, c=c: e.transpose(pT[:, c * 128:(c + 1) * 128], yan[i % 2][:, c * 128:(c + 1) * 128], identb),
                         reads=[("yan", i % 2), "identb"], writes=["pT"], inc=(c == 3))
                P.op("act", lambda e: e.copy(out=mixT[:, 4:8, i * 128:(i + 1) * 128], in_=pT[:, 0:512].rearrange("p (c t) -> p c t", c=4)),
                     reads=["pT"], writes=[("mixTa", i)])
            deferred.append([2, blk_norm])

    deferred = []
    for n in range(NU + 1):
        if n < NU:
            S1(n)
        if n >= 1:
            S2(n - 1)
            S3(n - 1)
            S4(n - 1)
        for d_ in deferred:
            d_[0] -= 1
        while deferred and deferred[0][0] <= 0:
            deferred.pop(0)[1]()
    while deferred:
        deferred.pop(0)[1]()
    if dbg:
        dbg_store("dbg_mixatt", mixT[:, 4:8, :], [("mixTa", i) for i in range(NT)], [128, 4 * T], view=lambda d: d.rearrange("p (c t) -> p c t", c=4))
    if upto <= 2:
        P.fence()
        return finish(nc, P, final_evs, out_d, dbg_d)

    for i in range(NT):
        for half in range(2):
            bank, bkey = next_bank()
            for kc in range(8):
                rk = [("mixT", kc, i // 4)] if kc < 4 else [("mixTa", i)]
                P.op("pe", lambda e, bank=bank, kc=kc, i=i, half=half: e.matmul(bank, mixT[:, kc, i * 128:(i + 1) * 128], wout[:, kc, half * 512:(half + 1) * 512],
                                                                                 start=(kc == 0), stop=(kc == 7)),
                     reads=rk + ["wout"], writes=[bkey], inc=(kc == 7))
            P.op("dve", lambda e, bank=bank, i=i, half=half: e.tensor_tensor(out=hbuf[:, i, half * 512:(half + 1) * 512], in0=bank,
                                                                              in1=hbuf[:, i, half * 512:(half + 1) * 512], op=ALU.add),
                 reads=[bkey, ("hbuf", i)], writes=[("hbuf", i)])
    if dbg:
        dbg_store("dbg_h1", hbuf, [("hbuf", i) for i in range(NT)], [128, NT * D], view=lambda d: d.rearrange("p (t f) -> p t f", t=NT))
    P.fence()
    if upto <= 3:
        return finish(nc, P, final_evs, out_d, dbg_d)


    xtok = sb_at("xtok", [128, NT, D], BF16, A0)
    cr = Arena(A0 + 32768, A1LO)
    wg = [cr.take("wg%d" % i, [128, 8, 512], BF16) for i in range(2)]
    wu = [cr.take("wu%d" % i, [128, 8, 512], BF16) for i in range(2)]
    wd = [cr.take("wd%d" % i, [128, 4, D], BF16) for i in range(2)]
    R2 = cr.cur
    c1 = Arena(R2, A1LO)
    xs2 = c1.take("xs2", [128, D], F32)
    xn2f = c1.take("xn2f", [128, 8, 128], F32)
    g2bc = c1.take("g2bc", [128, D], F32)
    c2 = Arena(A1LO + 65536, SB_HI)
    junk3 = c2.take("junk3", [128, D], BF16)
    gfbc = c2.take("gfbc", [128, D], F32)
    yst = [c2.take("yst%d" % i, [128, D], F32) for i in range(2)]
    RT = [c2.take("rt%d" % i, [128, 16, 16], F32) for i in range(6)]
    rs_ = [c2.take("rs%d" % i, [128, 16, 4], F32) for i in range(3)]
    rc = c2.take("rc", [128, 16 * 8], F32)
    ECc = c2.take("ECc", [128, 16, 16], F32)
    Mb = c2.take("Mb", [128, 256], BF16)
    UTb = c2.take("UTb", [128, 128], BF16)
    onesb = c2.take("onesb", [128, 128], BF16)
    offf = c2.take("offf", [128, 32], F32)
    offi = c2.take("offi", [128, 32], mybir.dt.int32)
    ones16 = pers.take("ones16", [128, 256], F32)

    P.dma("sp", lambda e: e.dma_start(out=g2bc, in_=g2_d), writes=["g2bc"])
    P.dma("sp", lambda e: e.dma_start(out=gfbc, in_=gf_d), writes=["gfbc"])
    P.dma("sp", lambda e: e.dma_start(out=ECc, in_=ec_d.rearrange("p (t e) -> p t e", t=16)), writes=["ECc"])
    P.dma("pool", lambda e: e.dma_start(out=UTb, in_=ut_d), writes=["UTb"])
    P.op("pool", lambda e: e.memset(onesb, 1.0), writes=["onesb"])
    P.op("pool", lambda e: e.memset(ones16, 1.0), writes=["ones16"])
    P.op("pool", lambda e: e.memset(yst[0], 0.0), writes=[("yst", 0)])
    P.dma("sp", lambda e: e.dma_start(out=Ys_d[NEXP * CAP:RROWS, :], in_=yst[0]), reads=[("yst", 0)], writes=["Ysdummy"])

    def load_expert(e_):
        s = e_ % 2
        P.dma("pool", lambda e: e.dma_start(out=wg[s], in_=weg_d[e_].rearrange("(kc p) n -> p kc n", p=128)), writes=[("wg", s)])
        P.dma("pool", lambda e: e.dma_start(out=wu[s], in_=weu_d[e_].rearrange("(kc p) n -> p kc n", p=128)), writes=[("wu", s)])
        P.dma("pool", lambda e: e.dma_start(out=wd[s], in_=wed_d[e_].rearrange("(kc p) n -> p kc n", p=128)), writes=[("wd", s)])

    if upto > 3.5:
        load_expert(0)
        load_expert(1)

    for i in range(NT):
        hk = ("hbuf", i)
        rms_stats(hbuf[:, i, :], hk, junk3, "junk3", 6, i, D)
        P.op("dve", lambda e, i=i: e.scalar_tensor_tensor(out=xs2, in0=hbuf[:, i, :], scalar=st(8, i), in1=g2bc, op0=ALU.mult, op1=ALU.mult),
             reads=[hk, ("stat", 6, i), "g2bc"], writes=["xs2"])
        P.op("pool", lambda e, i=i: e.tensor_copy(out=xtok[:, i, :], in_=xs2), reads=["xs2"], writes=[("xtok", i)])
        for c in range(8):
            P.op("pe", lambda e, c=c: e.transpose(pW[:, c * 128:(c + 1) * 128], xs2[:, c * 128:(c + 1) * 128], identf),
                 reads=["xs2", "identf"], writes=["W0", "W1"], inc=(c == 7))
        for hb, (bk, bkk) in enumerate(((W0, "W0"), (W1, "W1"))):
            P.op("act", lambda e, hb=hb, bk=bk: e.copy(out=xn2f[:, hb * 4:(hb + 1) * 4, :], in_=bk.rearrange("p (c t) -> p c t", c=4)),
                 reads=[bkk], writes=[("xn2f", hb)])
        for kc in range(8):
            P.op("pe", lambda e, kc=kc: e.matmul(pS[0][:, 0:20], xn2f[:, kc, :], wr[:, kc, :], start=(kc == 0), stop=(kc == 7)),
                 reads=[("xn2f", 0), ("xn2f", 1), "wr"], writes=["pS0"], inc=(kc == 7))
        P.op("dve", lambda e, i=i: e.tensor_tensor(out=logits[:, i, :], in0=pS[0][:, 0:20], in1=brbc, op=ALU.add),
             reads=["pS0", "brbc"], writes=["logits"])
    if upto <= 3.2:
        dbg_store("dbg_logits", logits, "logits", [128, NT * 20], view=lambda d: d.rearrange("p (t f) -> p t f", t=NT))
        P.fence()
        return finish(nc, P, final_evs, out_d, dbg_d)

    LG = logits[:, :, 0:4]
    LE = logits[:, :, 4:20]
    mg = rc[:, 0:16]
    sg_ = rc[:, 16:32]
    pg = rc[:, 32:48]
    m1 = rc[:, 48:64]
    m2 = rc[:, 64:80]
    w1 = rc[:, 80:96]
    w2 = rc[:, 96:112]
    e2 = rc[:, 112:128]
    ohg, tg_, eg = rs_
    ME, oh1, ME2, oh2, tA, tB = RT
    R = ["logits", "rc", "rs", "rt"]

    def rop(eng, fn, extra_r=()):
        P.op(eng, fn, reads=R + list(extra_r), writes=["rc", "rs", "rt"])

    def bc(ap2, n):
        return ap2.unsqueeze(2).to_broadcast([128, 16, n])

    rop("dve", lambda e: e.tensor_reduce(out=mg, in_=LG, axis=AX.X, op=ALU.max))
    rop("dve", lambda e: e.tensor_tensor(out=ohg, in0=LG, in1=bc(mg, 4), op=ALU.is_equal))
    rop("dve", lambda e: e.tensor_tensor(out=tg_, in0=LG, in1=bc(mg, 4), op=ALU.subtract))
    rop("act", lambda e: e.activation(out=eg, in_=tg_, func=AF.Exp))
    rop("dve", lambda e: e.tensor_reduce(out=sg_, in_=eg, axis=AX.X, op=ALU.add))
    rop("dve", lambda e: e.reciprocal(out=pg, in_=sg_))
    rop("dve", lambda e: e.tensor_scalar(out=tg_, in0=ohg, scalar1=1e30, scalar2=-1e30, op0=ALU.mult, op1=ALU.add))
    rop("dve", lambda e: e.tensor_tensor(out=ME.rearrange("p t (g k) -> p t g k", g=4), in0=LE.rearrange("p t (g k) -> p t g k", g=4),
                                         in1=tg_.unsqueeze(3).to_broadcast([128, 16, 4, 4]), op=ALU.add))
    rop("dve", lambda e: e.tensor_reduce(out=m1, in_=ME, axis=AX.X, op=ALU.max))
    rop("dve", lambda e: e.tensor_tensor(out=oh1, in0=ME, in1=bc(m1, 16), op=ALU.is_equal))
    rop("dve", lambda e: e.scalar_tensor_tensor(out=ME2, in0=oh1, scalar=-1e30, in1=ME, op0=ALU.mult, op1=ALU.add))
    rop("dve", lambda e: e.tensor_reduce(out=m2, in_=ME2, axis=AX.X, op=ALU.max))
    rop("dve", lambda e: e.tensor_tensor(out=oh2, in0=ME2, in1=bc(m2, 16), op=ALU.is_equal))
    rop("dve", lambda e: e.tensor_tensor(out=e2, in0=m2, in1=m1, op=ALU.subtract))
    rop("act", lambda e: e.activation(out=e2, in_=e2, func=AF.Exp))
    rop("dve", lambda e: e.tensor_scalar(out=w1, in0=e2, scalar1=1.0, scalar2=None, op0=ALU.add))
    rop("dve", lambda e: e.reciprocal(out=w1, in_=w1))
    rop("dve", lambda e: e.tensor_tensor(out=w2, in0=e2, in1=w1, op=ALU.mult))
    rop("dve", lambda e: e.tensor_tensor(out=w1, in0=w1, in1=pg, op=ALU.mult))
    rop("dve", lambda e: e.tensor_tensor(out=w2, in0=w2, in1=pg, op=ALU.mult))

    Mb_te = Mb.rearrange("p (e t) -> p t e", e=16)
    rop("dve", lambda e: e.tensor_tensor(out=Mb_te, in0=oh1, in1=oh2, op=ALU.add), ["Mb"])
    P.op("pe", lambda e: e.matmul(pS[0][:, 0:256], UTb, Mb, start=True, stop=True), reads=R + ["UTb", "Mb"], writes=["pS0"])
    P.op("pe", lambda e: e.matmul(pS[1][:, 0:256], onesb, Mb, start=True, stop=True), reads=R + ["onesb", "Mb"], writes=["pS1"])
    cntS = ME.rearrange("p t e -> p (t e)")
    incl = ME2.rearrange("p t e -> p (t e)")
    posb = tA.rearrange("p t e -> p (t e)")
    tmpb = tB
    rop("dve", lambda e: e.tensor_copy(out=cntS, in_=pS[1][:, 0:256]), ["pS1"])
    rop("dve", lambda e: e.tensor_tensor_scan(out=incl, data0=ones16, data1=cntS, initial=0.0, op0=ALU.mult, op1=ALU.add), ["ones16"])
    rop("dve", lambda e: e.tensor_tensor(out=posb, in0=incl, in1=cntS, op=ALU.subtract))
    rop("dve", lambda e: e.tensor_tensor(out=posb.rearrange("p (e t) -> p e t", e=16)[:, 1:16, :], in0=posb.rearrange("p (e t) -> p e t", e=16)[:, 1:16, :],
                                         in1=incl.rearrange("p (e t) -> p e t", e=16)[:, 0:15, 15:16].to_broadcast([128, 15, 16]), op=ALU.subtract))
    rop("dve", lambda e: e.tensor_tensor(out=posb, in0=posb, in1=pS[0][:, 0:256], op=ALU.add), ["pS0"])
    pos_te = posb.rearrange("p (e t) -> p t e", e=16)
    DUMC = cols[:, 44:45]
    for k, ohk in enumerate((oh1, oh2)):
        pk = offf[:, k * 16:(k + 1) * 16]
        ek = rc[:, 0:16]
        ov = rc[:, 16:32]
        rop("dve", lambda e, ohk=ohk: e.tensor_tensor(out=tmpb, in0=ohk, in1=pos_te, op=ALU.mult))
        rop("dve", lambda e, pk=pk: e.tensor_reduce(out=pk, in_=tmpb, axis=AX.X, op=ALU.add), ["offf"])
        rop("dve", lambda e, ohk=ohk: e.tensor_tensor(out=tmpb, in0=ohk, in1=ECc, op=ALU.mult), ["ECc"])
        rop("dve", lambda e, ek=ek: e.tensor_reduce(out=ek, in_=tmpb, axis=AX.X, op=ALU.add))
        rop("dve", lambda e, pk=pk, ov=ov: e.tensor_scalar(out=ov, in0=pk, scalar1=float(CAP), scalar2=None, op0=ALU.is_ge), ["offf"])
        rop("dve", lambda e, pk=pk, ek=ek: e.tensor_tensor(out=pk, in0=pk, in1=ek, op=ALU.add), ["offf"])
        rop("dve", lambda e, pk=pk, ek=ek: e.tensor_scalar(out=ek, in0=pk, scalar1=-1.0, scalar2=DUMC, op0=ALU.mult, op1=ALU.add), ["offf", "cols"])
        rop("dve", lambda e, ek=ek, ov=ov: e.tensor_tensor(out=ek, in0=ek, in1=ov, op=ALU.mult))
        rop("dve", lambda e, pk=pk, ek=ek: e.tensor_tensor(out=pk, in0=pk, in1=ek, op=ALU.add), ["offf"])
    P.op("dve", lambda e: e.tensor_copy(out=offi, in_=offf), reads=R + ["offf"], writes=["offi"])
    if dbg:
        dbg_store("dbg_logits", logits, "logits", [128, NT * 20], view=lambda d: d.rearrange("p (t f) -> p t f", t=NT))
        dbg_store("dbg_off", offf, ["offf", "rt"], [128, 32])
        dbg_store("dbg_w", rc[:, 80:112], ["rc", "rt", "offi"], [128, 32])
    if upto <= 3.5:
        P.fence()
        return finish(nc, P, final_evs, out_d, dbg_d)

    I32 = mybir.dt.int32
    xs_keys = []
    sc_evs = []
    for i in range(NT):
        for k in range(2):
            key = ("Xs", i, k)
            xs_keys.append(key)
            ev_ = P.dma("pool", lambda e, i=i, k=k: e.indirect_dma_start(
                out=Xs_d, out_offset=bass.IndirectOffsetOnAxis(ap=offi[:, k * 16 + i:k * 16 + i + 1], axis=0),
                in_=xtok[:, i, :], in_offset=None),
                reads=[("xtok", i), "offi"], writes=[key], extra=sc_evs[-4:-3])
            sc_evs.append(ev_)
    P.fence()
    yz = yst[0]
    ya_ = Arena(A0, A0 + 32768)
    NY = 6
    yst = [ya_.take("ystr%d" % i, [128, D], F32) for i in range(NY)]
    c3 = Arena(R2, A1LO)
    xe = [c3.take("xe0", [128, NB, D], BF16)] * 2
    xeT = [c3.take("xeT%d" % i, [128, 8, CAP], BF16) for i in range(2)]
    hidT = [c3.take("hidT0", [128, 4, CAP], BF16)] * 2
    sgt = [c3.take("sgt%d" % i, [128, CAP], BF16) for i in range(2)]

    GU = [(pS[0], "pS0", pS[1], "pS1"), (pS[2], "pS2", pS[3], "pS3")]
    gun = [0]
    dn = [0]
    Dbanks = [(W0, "W0"), (W1, "W1")]
    ys_keys = []
    nys = [0]
    nexp = NEXP if upto >= 5 else (max(1, int(round((upto - 4) * 100))) if upto > 4 else 1)
    tbanks = [(pT, "pT"), (pT2, "pT2")]
    tn = [0]

    def stage_T_load(e_):
        P.dma("sp", lambda e: e.dma_start(out=xe[0], in_=Xs_d[e_ * CAP:(e_ + 1) * CAP, :].rearrange("(b p) f -> p b f", p=128)),
              reads=xs_keys, writes=[("xe", 0)])

    def stage_T_block(e_, b):
        s = e_ % 2
        xek = ("xe", 0)
        tb_, tk_ = tbanks[tn[0] % 2]
        tn[0] += 1
        for kc in range(8):
            P.op("pe", lambda e, tb_=tb_, kc=kc: e.transpose(tb_[:, kc * 128:(kc + 1) * 128], xe[0][:, b, kc * 128:(kc + 1) * 128], identb),
                 reads=[xek, "identb"], writes=[tk_], inc=(kc == 7))
        evac_copy(xeT[s][:, :, b * 128:(b + 1) * 128], tb_.rearrange("p (c t) -> p c t", c=8), [tk_], [("xeT", s, b)])

    def stage_T(e_):
        stage_T_load(e_)
        for b in range(NB):
            stage_T_block(e_, b)

    def stage_GU(e_):
        s = e_ % 2
        xetk = [("xeT", s, b) for b in range(NB)]
        hid = hidT[s]
        for ffc in range(4):
            pg_, kg, pu_, ku = GU[gun[0] % 2]
            gun[0] += 1
            for kc in range(8):
                P.op("pe", lambda e, pg_=pg_, kc=kc, ffc=ffc, s=s: e.matmul(
                    pg_[:, 0:CAP], wg[s][:, kc, ffc * 128:(ffc + 1) * 128], xeT[s][:, kc, :], start=(kc == 0), stop=(kc == 7)),
                    reads=[("wg", s)] + xetk, writes=[kg], inc=(kc == 7))
            for kc in range(8):
                P.op("pe", lambda e, pu_=pu_, kc=kc, ffc=ffc, s=s: e.matmul(
                    pu_[:, 0:CAP], wu[s][:, kc, ffc * 128:(ffc + 1) * 128], xeT[s][:, kc, :], start=(kc == 0), stop=(kc == 7)),
                    reads=[("wu", s)] + xetk, writes=[ku], inc=(kc == 7))
            sg2 = sgt[ffc % 2]
            sk = "sgt%d" % (ffc % 2)
            P.op("act", lambda e, pg_=pg_, sg2=sg2: e.activation(out=sg2, in_=pg_[:, 0:CAP], func=AF.Silu), reads=[kg], writes=[sk])
            P.op("dve", lambda e, pu_=pu_, sg2=sg2, hid=hid, ffc=ffc: e.tensor_tensor(out=hid[:, ffc, :], in0=sg2, in1=pu_[:, 0:CAP], op=ALU.mult),
                 reads=[sk, ku], writes=[("hidT", 0, ffc)])

    def stage_D_block(e_, b):
        s = e_ % 2
        hid = hidT[s]
        yb = yst[nys[0] % NY]
        ybk = ("ystr", nys[0] % NY)
        nys[0] += 1
        for half in range(2):
            bank, bkey = Dbanks[dn[0] % 2]
            dn[0] += 1
            for ffc in range(4):
                P.op("pe", lambda e, bank=bank, hid=hid, ffc=ffc, half=half: e.matmul(
                    bank, hid[:, ffc, b * 128:(b + 1) * 128], wd[s][:, ffc, half * 512:(half + 1) * 512], start=(ffc == 0), stop=(ffc == 3)),
                    reads=[("hidT", 0, ffc), ("wd", s)], writes=[bkey], inc=(ffc == 3))
            evac_copy(yb[:, half * 512:(half + 1) * 512], bank, [bkey], [ybk])
        yk = ("Ys", e_, b)
        ys_keys.append(yk)
        P.dma("sp", lambda e, yb=yb: e.dma_start(out=Ys_d[e_ * CAP + b * 128:e_ * CAP + (b + 1) * 128, :], in_=yb),
              reads=[ybk], writes=[yk])

    stage_T(0)
    for e_ in range(nexp):
        stage_GU(e_)
        if e_ + 1 < nexp:
            stage_T_load(e_ + 1)
        for b in range(NB):
            stage_D_block(e_, b)
            if e_ + 1 < nexp:
                stage_T_block(e_ + 1, b)
        if e_ + 2 < nexp:
            load_expert(e_ + 2)

    P.fence()
    c4 = Arena(R2, A1LO)
    G = [c4.take("G%d" % i, [128, D], F32) for i in range(4)]
    ng = [0]
    def combine_acc(i):
        hk = ("hbuf", i)
        for k in range(2):
            gb = G[ng[0] % 4]
            gk = ("G", ng[0] % 4)
            ng[0] += 1
            P.dma("pool", lambda e, k=k, gb=gb: e.indirect_dma_start(
                out=gb, out_offset=None, in_=Ys_d,
                in_offset=bass.IndirectOffsetOnAxis(ap=offi[:, k * 16 + i:k * 16 + i + 1], axis=0)),
                reads=ys_keys + ["Ysdummy", "offi"], writes=[gk])
            wk_ = rc[:, 80 + k * 16 + i:80 + k * 16 + i + 1]
            P.op("dve", lambda e, gb=gb, wk_=wk_: e.scalar_tensor_tensor(out=hbuf[:, i, :], in0=gb, scalar=wk_, in1=hbuf[:, i, :],
                                                                        op0=ALU.mult, op1=ALU.add),
                 reads=[gk, "rc", hk], writes=[hk])
        rms_stats(hbuf[:, i, :], hk, junk3, "junk3", 9, i, D)

    def combine_fin(i):
        hk = ("hbuf", i)
        P.op("dve", lambda e: e.scalar_tensor_tensor(out=hbuf[:, i, :], in0=hbuf[:, i, :], scalar=st(11, i), in1=gfbc, op0=ALU.mult, op1=ALU.mult),
             reads=[hk, ("stat", 9, i), "gfbc"], writes=[hk])
        final_evs.append(P.dma("sp", lambda e: e.dma_start(out=out_d[i * 128:(i + 1) * 128, :], in_=hbuf[:, i, :]), reads=[hk]))

    for i in range(NT):
        combine_acc(i)
        if i >= 1:
            combine_fin(i - 1)
    combine_fin(NT - 1)
    return finish(nc, P, final_evs, out_d, dbg_d)


def finish(nc, P, final_evs, out_d, dbg_d):
    P.emit(final_waits=final_evs)
    return nc


def prep_shared(inp):
    f = np.float32
    rep = lambda v, n=128: np.ascontiguousarray(np.broadcast_to(np.asarray(v, f).reshape(1, -1), (n, np.asarray(v).size)))
    colsT = lambda v: np.ascontiguousarray(np.asarray(v, f).reshape(-1, 128).T)
    sh = {}
    sh["w_in"] = np.ascontiguousarray(inp["w_in"][0], f)
    sh["w_out"] = np.ascontiguousarray(inp["w_out"][0], f)
    sh["weg"] = np.ascontiguousarray(inp["w_e_gate"][0], f)
    sh["weu"] = np.ascontiguousarray(inp["w_e_up"][0], f)
    sh["wed"] = np.ascontiguousarray(inp["w_e_down"][0], f)
    sh["g1bc"] = rep(inp["norm1_g"][0])
    sh["g2bc"] = rep(inp["norm2_g"][0])
    sh["gfbc"] = rep(inp["final_g"])
    sh["gattbc"] = rep(inp["g_att_out"][0])
    cols = np.zeros((128, 64), f)
    cw = np.asarray(inp["conv_w"][0], f)
    for c in range(4):
        for j in range(4):
            cols[:, c * 4 + j] = cw[j, c * 128:(c + 1) * 128]
    cols[:, 16:20] = colsT(inp["conv_b"][0])
    cols[:, 20:24] = colsT(inp["b_rg_a"][0].reshape(-1))
    cols[:, 24:28] = colsT(inp["b_rg_x"][0].reshape(-1))
    cols[:, 28:32] = colsT(inp["lru_lambda"][0])
    cols[:, 32:36] = colsT(inp["g_rec_out"][0])
    rb = np.asarray(inp["rel_bias"][0], f)
    cols[:, 36:44] = np.broadcast_to(rb[:, 256][None, :], (128, 8))
    sh["cols"] = cols
    bd = np.zeros((128, 1024), f)
    wa = np.asarray(inp["w_rg_a"][0], f)
    wx = np.asarray(inp["w_rg_x"][0], f)
    for c in range(4):
        for hh in range(2):
            bd[hh * 64:(hh + 1) * 64, c * 128 + hh * 64: c * 128 + (hh + 1) * 64] = wa[2 * c + hh]
            bd[hh * 64:(hh + 1) * 64, 512 + c * 128 + hh * 64: 512 + c * 128 + (hh + 1) * 64] = wx[2 * c + hh]
    sh["bd"] = bd
    kk = np.arange(128)[:, None]
    qq = np.arange(128)[None, :]
    idx_prev = np.clip(128 + qq - kk, -128, 128) + 128
    idx_diag = np.clip(qq - kk, -128, 128) + 128
    masked = (kk >= 64) & (qq < 64)
    braw = np.zeros((128, 8, 256), f)
    for h in range(8):
        braw[:, h, 0:128] = rb[h][idx_prev]
        d = rb[h][idx_diag].copy()
        d[masked] = -30000.0
        braw[:, h, 128:256] = d
    sh["braw"] = braw.reshape(128, 2048)
    sh["wr"] = np.ascontiguousarray(np.concatenate([inp["w_group"][0], inp["w_router"][0]], axis=1), f)
    sh["brbc"] = rep(np.concatenate([inp["b_group"][0], inp["b_router"][0]]))
    sh["identf"] = np.eye(128, dtype=f)
    CAP = 512
    sh["ecc"] = np.ascontiguousarray(np.broadcast_to((np.arange(16, dtype=f) * CAP)[None, None, :], (128, 16, 16)).reshape(128, 256))
    sh["utri"] = np.triu(np.ones((128, 128), f), 1)
    cols[:, 44] = 16 * CAP + np.arange(128, dtype=f)
    return sh


_NC_CACHE = {}


def kernel(**inputs):
    inp = {k: np.asarray(v) for k, v in inputs.items()}
    sh = prep_shared(inp)
    if "nc" not in _NC_CACHE:
        _NC_CACHE["nc"] = build_nc()
    nc = _NC_CACHE["nc"]
    x = np.asarray(inp["x"], np.float32)
    in_maps = []
    for b in range(8):
        m = dict(sh)
        m["x"] = np.ascontiguousarray(x[b])
        in_maps.append(m)
    res = run_bass_kernel_spmd(nc, in_maps, core_ids=list(range(8)))
    out = np.stack([np.asarray(res.results[b]["out"], np.float32) for b in range(8)], axis=0)
    return out
```
